# Optimizing a Trainium2 kernel written in Bass

```python
import jax, jax.numpy as jnp
from jax import lax
import numpy as np

D_MODEL = 4096
BATCH = 4
SEQ = 2048
DEPTH = 1

HEAD_DIM = 128
N_HEADS = D_MODEL // HEAD_DIM
N_MOBA = N_HEADS // 2
N_FOX = N_HEADS - N_MOBA
D_MOBA = N_MOBA * HEAD_DIM
D_FOX = N_FOX * HEAD_DIM
D_IN = 3 * D_MOBA + 3 * D_FOX + N_FOX
MOBA_BLOCK = 256
MOBA_TOPK = 3
MOBA_QCHUNK = 16
FOX_QBLOCK = 128
PEER_HEADS = 8
PEER_NKEYS = 128
PEER_EXPERTS = PEER_NKEYS * PEER_NKEYS
PEER_TOPK = 16
PEER_DKEY = 256
PEER_HALF = PEER_DKEY // 2
PEER_TCHUNK = 128
N_MOD = 6
NORM_EPS = 1e-6
NEG_INF = -1e30

kernel_name = "hymba_moba_fox_peer_adaln"


def rms_norm(x, g):
    xf = x.astype(jnp.float32)
    y = xf * lax.rsqrt(jnp.mean(xf * xf, axis=-1, keepdims=True) + NORM_EPS)
    return (y * g.astype(jnp.float32)).astype(x.dtype)


def modulate(h, shift, scale):
    return h * (1 + scale[:, None, :]) + shift[:, None, :]


def split_heads(t, n):
    b, s, _ = t.shape
    return t.reshape(b, s, n, HEAD_DIM).transpose(0, 2, 1, 3)


def merge_heads(t):
    b, h, s, d = t.shape
    return t.transpose(0, 2, 1, 3).reshape(b, s, h * d)


def alibi_slopes(n):
    return 2.0 ** (-8.0 * jnp.arange(1, n + 1, dtype=jnp.float32) / n)


def moba_attention(q, k, v, slopes):
    B, H, S, hd = q.shape
    nb = -(-S // MOBA_BLOCK)
    sp = nb * MOBA_BLOCK
    pad = ((0, 0), (0, 0), (0, sp - S), (0, 0))
    kp = jnp.pad(k, pad)
    vp = jnp.pad(v, pad)
    counts = np.minimum(MOBA_BLOCK, S - np.arange(nb) * MOBA_BLOCK).astype(np.float32)
    kmean = kp.reshape(B, H, nb, MOBA_BLOCK, hd).astype(jnp.float32).sum(3) / counts[:, None]
    qblk = jnp.arange(S) // MOBA_BLOCK
    gate = jnp.einsum('bhsd,bhnd->bhsn', q.astype(jnp.float32), kmean)
    past = jnp.arange(nb)[None, :] < qblk[:, None]
    gate = jnp.where(past, gate, NEG_INF)
    ksel = min(MOBA_TOPK, nb)
    _, sel = lax.top_k(gate, ksel)
    sel_valid = sel < qblk[:, None]
    scale = HEAD_DIM ** -0.5
    offs = jnp.arange(MOBA_BLOCK)
    bidx = jnp.arange(B)[:, None, None]
    hidx = jnp.arange(H)[None, :, None]
    slope = slopes[:, None, None]

    def chunk(ci):
        t0 = ci * MOBA_QCHUNK
        qc = lax.dynamic_slice_in_dim(q, t0, MOBA_QCHUNK, axis=2)
        tpos = t0 + jnp.arange(MOBA_QCHUNK)
        b0 = (t0 // MOBA_BLOCK) * MOBA_BLOCK
        k_own = lax.dynamic_slice_in_dim(kp, b0, MOBA_BLOCK, axis=2)
        v_own = lax.dynamic_slice_in_dim(vp, b0, MOBA_BLOCK, axis=2)
        spos_own = b0 + offs
        dist_own = (tpos[:, None] - spos_own[None, :]).astype(jnp.float32)
        l_own = jnp.einsum('bhqd,bhkd->bhqk', qc, k_own).astype(jnp.float32) * scale - slope * dist_own
        l_own = jnp.where(spos_own[None, :] <= tpos[:, None], l_own, NEG_INF)
        sel_c = lax.dynamic_slice_in_dim(sel, t0, MOBA_QCHUNK, axis=2)
        val_c = lax.dynamic_slice_in_dim(sel_valid, t0, MOBA_QCHUNK, axis=2)
        spos_sel = (sel_c[..., None] * MOBA_BLOCK + offs).reshape(B, H, MOBA_QCHUNK, ksel * MOBA_BLOCK)
        flat = spos_sel.reshape(B, H, MOBA_QCHUNK * ksel * MOBA_BLOCK)
        k_sel = kp[bidx, hidx, flat].reshape(B, H, MOBA_QCHUNK, ksel * MOBA_BLOCK, hd)
        v_sel = vp[bidx, hidx, flat].reshape(B, H, MOBA_QCHUNK, ksel * MOBA_BLOCK, hd)
        dist_sel = (tpos[:, None] - spos_sel).astype(jnp.float32)
        l_sel = jnp.einsum('bhqd,bhqkd->bhqk', qc, k_sel).astype(jnp.float32) * scale - slope * dist_sel
        l_sel = jnp.where(jnp.repeat(val_c, MOBA_BLOCK, axis=-1), l_sel, NEG_INF)
        p = jax.nn.softmax(jnp.concatenate([l_own, l_sel], axis=-1), axis=-1).astype(v.dtype)
        return (jnp.einsum('bhqk,bhkd->bhqd', p[..., :MOBA_BLOCK], v_own)
                + jnp.einsum('bhqk,bhqkd->bhqd', p[..., MOBA_BLOCK:], v_sel))

    outs = lax.map(chunk, jnp.arange(S // MOBA_QCHUNK))
    return outs.transpose(1, 2, 0, 3, 4).reshape(B, H, S, hd)


def forgetting_attention(q, k, v, log_f):
    B, H, S, hd = q.shape
    F = jnp.cumsum(log_f, axis=-1)
    spos = jnp.arange(S)
    scale = HEAD_DIM ** -0.5

    def block(bi):
        t0 = bi * FOX_QBLOCK
        qb = lax.dynamic_slice_in_dim(q, t0, FOX_QBLOCK, axis=2)
        Fq = lax.dynamic_slice_in_dim(F, t0, FOX_QBLOCK, axis=2)
        tpos = t0 + jnp.arange(FOX_QBLOCK)
        l = jnp.einsum('bhqd,bhkd->bhqk', qb, k).astype(jnp.float32) * scale + Fq[..., None] - F[:, :, None, :]
        l = jnp.where(spos[None, :] <= tpos[:, None], l, NEG_INF)
        p = jax.nn.softmax(l, axis=-1).astype(v.dtype)
        return jnp.einsum('bhqk,bhkd->bhqd', p, v)

    outs = lax.map(block, jnp.arange(S // FOX_QBLOCK))
    return outs.transpose(1, 2, 0, 3, 4).reshape(B, H, S, hd)


def peer_ffn(h, w_pq, sub_keys, expert_u, expert_v):
    B, S, D = h.shape
    q = (h @ w_pq).reshape(B, S, PEER_HEADS, 2, PEER_HALF)
    s = jnp.einsum('bshcd,hcnd->bshcn', q, sub_keys).astype(jnp.float32)
    sv, si = lax.top_k(s, PEER_TOPK)
    cand = (sv[..., 0, :, None] + sv[..., 1, None, :]).reshape(B, S, PEER_HEADS, PEER_TOPK * PEER_TOPK)
    cidx = (si[..., 0, :, None] * PEER_NKEYS + si[..., 1, None, :]).reshape(B, S, PEER_HEADS, PEER_TOPK * PEER_TOPK)
    top_s, top_c = lax.top_k(cand, PEER_TOPK)
    eidx = jnp.take_along_axis(cidx, top_c, axis=-1)
    g = jax.nn.softmax(top_s, axis=-1)
    n = (B * S) // PEER_TCHUNK
    kk = PEER_HEADS * PEER_TOPK
    hf = h.reshape(n, PEER_TCHUNK, D)
    ef = eidx.reshape(n, PEER_TCHUNK, kk)
    gf = g.reshape(n, PEER_TCHUNK, kk)

    def chunk(args):
        hc, ec, gc = args
        u = expert_u[ec]
        a = jax.nn.gelu(jnp.einsum('td,tkd->tk', hc, u).astype(jnp.float32), approximate=False)
        w = (gc * a).astype(h.dtype)
        return jnp.einsum('tk,tkd->td', w, expert_v[ec])

    y = lax.map(chunk, (hf, ef, gf))
    return y.reshape(B, S, D)


def setup_inputs(seed: int = 0) -> dict:
    key = jax.random.key(seed)
    ks = jax.random.split(key, 18)
    f32 = jnp.float32
    D = D_MODEL
    nrm = lambda k, shp, s: jax.random.normal(k, shp, f32) * s
    return {
        "x": nrm(ks[0], (BATCH, SEQ, D), 1.0),
        "c": nrm(ks[1], (BATCH, D), 1.0),
        "w_ada": nrm(ks[2], (DEPTH, D, N_MOD * D), 0.5 * D ** -0.5),
        "b_ada": nrm(ks[3], (DEPTH, N_MOD * D), 0.01),
        "norm1_g": 1.0 + nrm(ks[4], (DEPTH, D), 0.02),
        "w_in": nrm(ks[5], (DEPTH, D, D_IN), D ** -0.5),
        "b_f": jax.random.uniform(ks[6], (DEPTH, N_FOX), f32, 1.0, 6.0),
        "q_norm_moba": 1.0 + nrm(ks[7], (DEPTH, HEAD_DIM), 0.02),
        "k_norm_moba": 1.0 + nrm(ks[8], (DEPTH, HEAD_DIM), 0.02),
        "q_norm_fox": 1.0 + nrm(ks[9], (DEPTH, HEAD_DIM), 0.02),
        "k_norm_fox": 1.0 + nrm(ks[10], (DEPTH, HEAD_DIM), 0.02),
        "w_out": nrm(ks[11], (DEPTH, D, D), D ** -0.5),
        "norm2_g": 1.0 + nrm(ks[12], (DEPTH, D), 0.02),
        "w_pq": nrm(ks[13], (DEPTH, D, PEER_HEADS * PEER_DKEY), D ** -0.5),
        "peer_sub_keys": nrm(ks[14], (DEPTH, PEER_HEADS, 2, PEER_NKEYS, PEER_HALF), PEER_HALF ** -0.5),
        "peer_u": nrm(ks[15], (DEPTH, PEER_EXPERTS, D), D ** -0.5),
        "peer_v": nrm(ks[16], (DEPTH, PEER_EXPERTS, D), PEER_HEADS ** -0.5),
    }


def reference(x, c, w_ada, b_ada, norm1_g, w_in, b_f, q_norm_moba, k_norm_moba,
              q_norm_fox, k_norm_fox, w_out, norm2_g, w_pq, peer_sub_keys, peer_u, peer_v):
    B, S, D = x.shape
    slopes = alibi_slopes(N_MOBA)
    c_act = jax.nn.silu(c)
    cuts = [D_MOBA, 2 * D_MOBA, 3 * D_MOBA, 3 * D_MOBA + D_FOX, 3 * D_MOBA + 2 * D_FOX, 3 * D_MOBA + 3 * D_FOX]
    for l in range(DEPTH):
        mod = (c_act @ w_ada[l] + b_ada[l]).reshape(B, N_MOD, D)
        sh1, sc1, g1 = mod[:, 0], mod[:, 1], mod[:, 2]
        sh2, sc2, g2 = mod[:, 3], mod[:, 4], mod[:, 5]

        h = modulate(rms_norm(x, norm1_g[l]), sh1, sc1)
        proj = h @ w_in[l]
        qa, ka, va, qf, kf, vf, fg = jnp.split(proj, cuts, axis=-1)
        qa = rms_norm(split_heads(qa, N_MOBA), q_norm_moba[l])
        ka = rms_norm(split_heads(ka, N_MOBA), k_norm_moba[l])
        va = split_heads(va, N_MOBA)
        qf = rms_norm(split_heads(qf, N_FOX), q_norm_fox[l])
        kf = rms_norm(split_heads(kf, N_FOX), k_norm_fox[l])
        vf = split_heads(vf, N_FOX)
        log_f = jax.nn.log_sigmoid((fg + b_f[l]).astype(jnp.float32)).transpose(0, 2, 1)
        o_moba = moba_attention(qa, ka, va, slopes)
        o_fox = forgetting_attention(qf, kf, vf, log_f)
        mix = jnp.concatenate([merge_heads(o_moba), merge_heads(o_fox)], axis=-1)
        x = x + g1[:, None, :] * (mix @ w_out[l])

        h2 = modulate(rms_norm(x, norm2_g[l]), sh2, sc2)
        x = x + g2[:, None, :] * peer_ffn(h2, w_pq[l], peer_sub_keys[l], peer_u[l], peer_v[l])
    return x
```

```python
import numpy as np
import os
from contextlib import ExitStack
import concourse.bass as bass
import concourse.mybir as mybir
from concourse.bass_utils import run_bass_kernel_spmd

F32 = mybir.dt.float32
BF16 = mybir.dt.bfloat16
AF = mybir.ActivationFunctionType
ALU = mybir.AluOpType
AX = mybir.AxisListType

D = 4096
KC = 32
S_ALL = 2048
S_Q = 1024
NEG = -30000.0
EPS = 1e-6
OFF = [r * (r + 1) for r in range(9)]


class Res:
    __slots__ = ("name", "lw", "rd")

    def __init__(self, name=""):
        self.name = name
        self.lw = None
        self.rd = {}


class Sched:
    def __init__(self, nc, ndma=8):
        self.nc = nc
        self.eng = {"pe": nc.tensor, "act": nc.scalar, "dve": nc.vector, "pool": nc.gpsimd, "sp": nc.sync}
        self.sem = {}
        self.cnt = {}
        self.seen = {e: {} for e in self.eng}
        for e in ("pe", "act", "dve", "pool"):
            self.sem[e] = nc.alloc_semaphore(name="c_" + e)
            self.cnt[e] = 0
        self.dpool = {}
        self.dnext = {}
        for q in ("sp", "pool"):
            self.dpool[q] = []
            for i in range(ndma):
                k = "d_%s%d" % (q, i)
                self.sem[k] = nc.alloc_semaphore(name=k)
                self.cnt[k] = 0
                self.dpool[q].append(k)
            self.dnext[q] = 0
        self.nins = 0

    def _wait(self, e, key, val):
        if val <= 0 or self.seen[e].get(key, 0) >= val:
            return
        self.eng[e].wait_ge(self.sem[key], val)
        self.seen[e][key] = val
        self.nins += 1

    def _deps(self, e, r, w):
        deps = {}
        for x in r:
            if x.lw is not None:
                k, v = x.lw
                if deps.get(k, 0) < v:
                    deps[k] = v
        for x in w:
            if x.lw is not None:
                k, v = x.lw
                if deps.get(k, 0) < v:
                    deps[k] = v
            for k, v in x.rd.items():
                if deps.get(k, 0) < v:
                    deps[k] = v
        for k, v in deps.items():
            if k == "pe" and e == "pe":
                continue
            self._wait(e, k, v)

    def _mark(self, key, val, r, w):
        for x in r:
            if x.rd.get(key, 0) < val:
                x.rd[key] = val
        for x in w:
            x.lw = (key, val)
            x.rd = {}

    def op(self, e, fn, r=(), w=()):
        self._deps(e, r, w)
        ins = fn(self.eng[e])
        self.cnt[e] += 1
        ins.then_inc(self.sem[e], 1)
        self._mark(e, self.cnt[e], r, w)
        self.nins += 1
        return ins

    def dma(self, q, out, in_, r=(), w=(), **kw):
        k = self.dpool[q][self.dnext[q] % len(self.dpool[q])]
        self.dnext[q] += 1
        self._wait(q, k, self.cnt[k])
        self._deps(q, r, w)
        ins = self.eng[q].dma_start(out=out, in_=in_, **kw)
        self.cnt[k] += 16
        ins.then_inc(self.sem[k], 16)
        self._mark(k, self.cnt[k], r, w)
        self.nins += 1
        return ins

    def barrier(self):
        for e in self.eng:
            for k, v in self.cnt.items():
                if k == e and e == "pe":
                    continue
                self._wait(e, k, v)

    def finish(self, e="sp"):
        for k, v in self.cnt.items():
            self._wait(e, k, v)


def gtile(r, hf):
    if hf == 0:
        return 2 * r if r % 2 == 0 else 2 * r + 1
    return 2 * r + 1 if r % 2 == 0 else 2 * r


def build(debug=False, stop_after=99):
    nc = bass.Bass("TRN2", target_bir_lowering=False)
    S = Sched(nc)

    def din(name, shape, dt=F32):
        return nc.dram_tensor(name, shape, dt, kind="ExternalInput").ap()

    def dscr(name, shape, dt):
        return nc.dram_tensor(name, shape, dt, kind="ExternalOutput" if debug else "Internal").ap()

    xall = din("xall", [S_ALL, D])
    xq = din("xq", [S_Q, D])
    cT = din("cT", [128, KC])
    w_ada = din("w_ada", [D, 6 * D])
    b_ada = din("b_ada", [1, 6 * D])
    n1g = din("n1g", [128, KC])
    n2g = din("n2g", [128, KC])
    w_in = din("w_in", [D, 12304])
    bfrep = din("bfrep", [128, 16])
    gains = din("gains", [128, 4])
    w_out = din("w_out", [D, D])
    w_pq = din("w_pq", [D, 2048])
    skT = din("skT", [128, 2048])
    uT = din("uT", [D, 16384])
    pv = din("pv", [16384, D])
    albias = din("albias", [128, 16 * 72])
    seld = din("sel", [128, 128])
    maskd = din("maskb", [128, 2048])
    pastd = din("pastb", [128, 64])
    End = din("En", [8, 1024])
    out = nc.dram_tensor("out", [S_Q, D], F32, kind="ExternalOutput").ap()

    mod_d = dscr("mod_d", [192, 128], F32)
    KT_d = dscr("KT_d", [32, 128, S_ALL], BF16)
    QT_d = dscr("QT_d", [32, 128, S_Q], BF16)
    V_d = dscr("V_d", [S_ALL, D], BF16)
    lf_d = dscr("lf_d", [S_ALL, 16], F32)
    x1_d = dscr("x1_d", [S_Q, D], F32)
    WgT_d = dscr("WgT_d", [16384, S_Q], BF16)
    R_scr = {k: Res(k) for k in ["mod_d", "KT_d", "QT_d", "V_d", "lf_d", "x1_d", "WgT_d", "out"]}

    w_ada_v = w_ada.rearrange("(kc p) n -> p kc n", p=128)
    w_in_v = w_in.rearrange("(kc p) n -> p kc n", p=128)
    w_out_v = w_out.rearrange("(kc p) n -> p kc n", p=128)
    w_pq_v = w_pq.rearrange("(kc p) n -> p kc n", p=128)
    uT_v = uT.rearrange("(kc p) n -> p kc n", p=128)

    def wload(dst, src_v, c0, cw, res, nsplit=4):
        step = KC // nsplit
        for i in range(nsplit):
            S.dma("pool", dst[:, i * step:(i + 1) * step, 0:cw], src_v[:, i * step:(i + 1) * step, c0:c0 + cw], w=[res])

    with ExitStack() as gs:
        def GT(name, shape, dt):
            return gs.enter_context(nc.sbuf_tensor(name, shape, dt))

        identf = GT("identf", [128, 128], F32)
        identb = GT("identb", [128, 128], BF16)
        onesb = GT("onesb", [128, 128], BF16)
        modT = GT("modT", [128, 192], F32)
        A1 = GT("A1", [128, KC], F32)
        A2 = GT("A2", [128, KC], F32)
        gn = GT("gn", [128, 2 * KC], F32)
        gainsb = GT("gainsb", [128, 4], F32)
        cb = GT("cb", [128, KC], BF16)
        r_c = Res("c")
        r_const = Res("const")
        r_mod = Res("modT")

        def ada_group(ng, wa_b, brow_b, orow_b, psrow, r_wa_b, r_brow_b, r_orow_b, r_ps_b):
            S.dma("sp", brow_b[:], b_ada[0:1, ng * 512:(ng + 1) * 512], w=[r_brow_b])
            for kc in range(KC):
                S.op("pe", lambda e, kc=kc: e.matmul(psrow, lhsT=cb[:, kc:kc + 1], rhs=wa_b[:, kc, :],
                                                     start=(kc == 0), stop=(kc == KC - 1)), r=[r_c, r_wa_b], w=[r_ps_b])
            S.op("dve", lambda e: e.tensor_tensor(out=orow_b[:], in0=psrow, in1=brow_b[:], op=ALU.add),
                 r=[r_ps_b, r_brow_b], w=[r_orow_b])
            S.dma("sp", mod_d[ng * 4:(ng + 1) * 4, :].rearrange("(o j) p -> o (j p)", o=1), orow_b[:],
                  r=[r_orow_b], w=[R_scr["mod_d"]])

        def build_modT(lo, hi, tag):
            with ExitStack() as esm:
                mrow = esm.enter_context(nc.sbuf_tensor("mrow" + tag, [64, 256], F32))
                psm = esm.enter_context(nc.psum_tensor("psm" + tag, [128, 128], F32))
                r_m = Res()
                n = (hi - lo) // 64
                for i in range(n):
                    S.dma("sp", mrow[:, i * 128:(i + 1) * 128], mod_d[lo + i * 64:lo + (i + 1) * 64, :], r=[R_scr["mod_d"]], w=[r_m])
                for i in range(n):
                    S.op("pe", lambda e, i=i: e.matmul(psm[:, i * 64:(i + 1) * 64], lhsT=mrow[:, i * 128:(i + 1) * 128],
                                                       rhs=identf[0:64, 0:64], start=True, stop=True), r=[r_m, r_const], w=[r_mod])
                S.op("dve", lambda e: e.tensor_copy(out=modT[:, lo:hi], in_=psm[:, 0:hi - lo]), r=[r_mod], w=[r_mod])
                S.barrier()

        S.op("pool", lambda e: e.memset(identf[:], 0.0), w=[r_const])
        S.op("pool", lambda e: e.affine_select(out=identf[:], in_=identf[:], pattern=[[-1, 128]], compare_op=ALU.not_equal,
                                               fill=1.0, base=0, channel_multiplier=1), r=[r_const], w=[r_const])
        S.op("dve", lambda e: e.tensor_copy(out=identb[:], in_=identf[:]), r=[r_const], w=[r_const])
        S.op("dve", lambda e: e.memset(onesb[:], 1.0), w=[r_const])
        S.dma("sp", gn[:, 0:KC], n1g[:, :], w=[r_const])
        S.dma("sp", gn[:, KC:2 * KC], n2g[:, :], w=[r_const])
        S.dma("sp", gainsb[:], gains[:, :], w=[r_const])

        with ExitStack() as es:
            def T(name, shape, dt):
                return es.enter_context(nc.sbuf_tensor(name, shape, dt))

            def P(name, shape, dt):
                return es.enter_context(nc.psum_tensor(name, shape, dt))

            cf = T("cf", [128, KC], F32)
            wa = [T("wa%d" % i, [128, KC, 512], BF16) for i in range(2)]
            brow = [T("brow%d" % i, [1, 512], F32) for i in range(2)]
            orow = [T("orow%d" % i, [1, 512], F32) for i in range(2)]
            psr = [P("psr%d" % i, [1, 512], F32) for i in range(2)]
            r_wa = [Res(), Res()]
            r_brow = [Res(), Res()]
            r_orow = [Res(), Res()]
            r_psr = [Res(), Res()]
            S.dma("sp", cf[:], cT[:, :], w=[r_c])
            S.op("act", lambda e: e.activation(out=cb[:], in_=cf[:], func=AF.Silu), r=[r_c], w=[r_c])
            NG0 = 16
            wload(wa[0], w_ada_v, 0, 512, r_wa[0])
            for ng in range(NG0):
                b = ng % 2
                if ng + 1 < NG0:
                    wload(wa[1 - b], w_ada_v, (ng + 1) * 512, 512, r_wa[1 - b])
                ada_group(ng, wa[b], brow[b], orow[b], psr[b][0:1, :], r_wa[b], r_brow[b], r_orow[b], r_psr[b])
            S.barrier()
        build_modT(0, 64, "a")
        S.op("dve", lambda e: e.scalar_tensor_tensor(out=A1[:], in0=modT[:, KC:2 * KC], scalar=1.0, in1=gn[:, 0:KC],
                                                     op0=ALU.add, op1=ALU.mult), r=[r_mod, r_const], w=[r_mod])
        S.barrier()
        if stop_after <= 0:
            S.finish()
            return nc

        def build_hT(es, hT, r_hT, src, row0, Acol, shcol, tagname):
            def T(name, shape, dt):
                return es.enter_context(nc.sbuf_tensor(tagname + name, shape, dt))

            def P(name, shape, dt):
                return es.enter_context(nc.psum_tensor(tagname + name, shape, dt))

            xt = [T("xt%d" % i, [128, D], F32) for i in range(2)]
            xn = [T("xn%d" % i, [128, D], BF16) for i in range(2)]
            junk = T("junk", [128, D], BF16)
            st = T("st", [128, 16], F32)
            pst = [P("pst%d" % i, [128, 1024], BF16) for i in range(2)]
            r_xt = [Res(), Res()]
            r_xn = [Res(), Res()]
            r_junk = Res()
            r_st = Res()
            r_pst = [Res(), Res()]
            S.dma("sp", xt[0][:], src[row0:row0 + 128, :], w=[r_xt[0]])
            k = 0
            for tt in range(8):
                b = tt % 2
                if tt + 1 < 8:
                    S.dma("sp", xt[1 - b][:], src[row0 + (tt + 1) * 128:row0 + (tt + 2) * 128, :], w=[r_xt[1 - b]])
                S.op("dve", lambda e, b=b: e.memset(st[:, 2 * b:2 * b + 1], 0.0), w=[r_st])
                S.op("act", lambda e, b=b, tt=tt: e.activation(out=junk[:], in_=xt[b][:], func=AF.Square, accum_out=st[:, 2 * b:2 * b + 1]),
                     r=[r_xt[b]], w=[r_junk, r_st])
                S.op("act", lambda e, b=b: e.activation(out=st[:, 2 * b + 1:2 * b + 2], in_=st[:, 2 * b:2 * b + 1], func=AF.Sqrt,
                                                        bias=EPS, scale=1.0 / D), r=[r_st], w=[r_st])
                S.op("dve", lambda e, b=b: e.reciprocal(out=st[:, 2 * b + 1:2 * b + 2], in_=st[:, 2 * b + 1:2 * b + 2]), r=[r_st], w=[r_st])
                S.op("dve", lambda e, b=b: e.tensor_scalar(out=xn[b][:], in0=xt[b][:], scalar1=st[:, 2 * b + 1:2 * b + 2], scalar2=None,
                                                           op0=ALU.mult), r=[r_xt[b], r_st], w=[r_xn[b]])
                for q4 in range(4):
                    pb = k % 2
                    k += 1
                    for i in range(8):
                        kc = q4 * 8 + i
                        S.op("pe", lambda e, pb=pb, i=i, kc=kc, b=b: e.transpose(out=pst[pb][:, i * 128:(i + 1) * 128],
                                                                                 in_=xn[b][:, kc * 128:(kc + 1) * 128], identity=identb[:]),
                             r=[r_xn[b], r_const], w=[r_pst[pb]])
                    for i in range(8):
                        kc = q4 * 8 + i
                        if False:
                            S.op("act", lambda e, pb=pb, i=i, kc=kc, tt=tt: e.activation(
                                out=hT[:, kc, tt * 128:(tt + 1) * 128], in_=pst[pb][:, i * 128:(i + 1) * 128], func=AF.Identity,
                                bias=shcol[:, kc:kc + 1], scale=Acol[:, kc:kc + 1]),
                                 r=[r_pst[pb], r_mod], w=[r_hT[tt][kc]])
                        else:
                            S.op("dve", lambda e, pb=pb, i=i, kc=kc, tt=tt: e.tensor_scalar(
                                out=hT[:, kc, tt * 128:(tt + 1) * 128], in0=pst[pb][:, i * 128:(i + 1) * 128],
                                scalar1=Acol[:, kc:kc + 1], scalar2=shcol[:, kc:kc + 1], op0=ALU.mult, op1=ALU.add),
                                 r=[r_pst[pb], r_mod], w=[r_hT[tt][kc]])
            S.barrier()

        passes = [(xall, 0, 0, "kv"), (xall, 1024, 1024, "kv"), (xq, 0, 0, "q")]
        for pi, (src, row0, tok0, kind) in enumerate(passes):
            with ExitStack() as es:
                def T(name, shape, dt):
                    return es.enter_context(nc.sbuf_tensor("p%d_%s" % (pi, name), shape, dt))

                def P(name, shape, dt):
                    return es.enter_context(nc.psum_tensor("p%d_%s" % (pi, name), shape, dt))

                hT = T("hT", [128, KC, 1024], BF16)
                r_hT = [[Res() for _ in range(KC)] for _ in range(8)] if os.environ.get('FINE_HT', '1') == '1' else [[Res()] * KC for _ in range(8)]
                wg = [T("wg%d" % i, [128, KC, 512], BF16) for i in range(2)]
                r_wg = [Res(), Res()]
                first_c0 = 2048 if kind == "kv" else 0
                wload(wg[0], w_in_v, first_c0, 512, r_wg[0])
                with ExitStack() as es2:
                    build_hT(es2, hT, r_hT, src, row0, A1, modT[:, 0:KC], "p%d_" % pi)
                psk = [P("psk%d" % i, [128, 512], F32) for i in range(2)]
                pss = [P("pss%d" % i, [128, 512], F32) for i in range(2)]
                r_psk = [Res(), Res()]
                r_pss = [Res(), Res()]
                sq = [T("sq%d" % i, [128, 512], BF16) for i in range(2)]
                sd = [T("sd%d" % i, [128, 512], F32) for i in range(2)]
                kn = [T("kn%d" % i, [128, 512], BF16) for i in range(2)]
                r_sq = [Res(), Res()]
                r_sd = [Res(), Res()]
                r_kn = [Res(), Res()]
                vb = [T("vb%d" % i, [128, 512], BF16) for i in range(2)]
                r_vb = [Res(), Res()]
                groups = []
                if kind == "kv":
                    for g in range(4):
                        groups.append((2048 + g * 512, "k", g * 4, 1))
                    for g in range(4):
                        groups.append((8192 + g * 512, "k", 16 + g * 4, 3))
                    for g in range(4):
                        groups.append((4096 + g * 512, "v", g * 4, 0))
                    for g in range(4):
                        groups.append((10240 + g * 512, "v", 16 + g * 4, 0))
                else:
                    for g in range(4):
                        groups.append((g * 512, "q", g * 4, 0))
                    for g in range(4):
                        groups.append((6144 + g * 512, "q", 16 + g * 4, 2))
                assert groups[0][0] == first_c0
                u = 0
                vcount = 0
                for gi, (c0, typ, head0, gcol) in enumerate(groups):
                    b = gi % 2
                    if gi + 1 < len(groups):
                        wload(wg[1 - b], w_in_v, groups[gi + 1][0], 512, r_wg[1 - b])
                    if typ in ("k", "q"):
                        dst = KT_d if typ == "k" else QT_d
                        rdst = R_scr["KT_d"] if typ == "k" else R_scr["QT_d"]
                        for hh in range(4):
                            for tg in range(2):
                                pb = u % 2
                                u += 1
                                for kc in range(KC):
                                    S.op("pe", lambda e, pb=pb, kc=kc, hh=hh, tg=tg, b=b: e.matmul(
                                        psk[pb][:], lhsT=wg[b][:, kc, hh * 128:(hh + 1) * 128], rhs=hT[:, kc, tg * 512:(tg + 1) * 512],
                                        start=(kc == 0), stop=(kc == KC - 1)),
                                         r=[r_wg[b]] + [r_hT[t_][kc] for t_ in range(tg * 4, tg * 4 + 4)], w=[r_psk[pb]])
                                S.op("act", lambda e, pb=pb: e.activation(out=sq[pb][:], in_=psk[pb][:], func=AF.Square),
                                     r=[r_psk[pb]], w=[r_sq[pb]])
                                S.op("pe", lambda e, pb=pb: e.matmul(pss[pb][:], lhsT=onesb[:], rhs=sq[pb][:], start=True, stop=True),
                                     r=[r_sq[pb], r_const], w=[r_pss[pb]])
                                S.op("act", lambda e, pb=pb: e.activation(out=sd[pb][:], in_=pss[pb][:], func=AF.Sqrt, bias=EPS, scale=1.0 / 128),
                                     r=[r_pss[pb]], w=[r_sd[pb]])
                                S.op("dve", lambda e, pb=pb: e.reciprocal(out=sd[pb][:], in_=sd[pb][:]), r=[r_sd[pb]], w=[r_sd[pb]])
                                S.op("dve", lambda e, pb=pb, gcol=gcol: e.scalar_tensor_tensor(
                                    out=kn[pb][:], in0=psk[pb][:], scalar=gainsb[:, gcol:gcol + 1], in1=sd[pb][:], op0=ALU.mult, op1=ALU.mult),
                                     r=[r_psk[pb], r_sd[pb], r_const], w=[r_kn[pb]])
                                t0 = tok0 + tg * 512
                                S.dma("sp", dst[head0 + hh, :, t0:t0 + 512], kn[pb][:], r=[r_kn[pb]], w=[rdst])
                    else:
                        for tt in range(8):
                            pb = u % 2
                            u += 1
                            for kc in range(KC):
                                S.op("pe", lambda e, pb=pb, kc=kc, tt=tt, b=b: e.matmul(
                                    psk[pb][:], lhsT=hT[:, kc, tt * 128:(tt + 1) * 128], rhs=wg[b][:, kc, :],
                                    start=(kc == 0), stop=(kc == KC - 1)), r=[r_wg[b], r_hT[tt][kc]], w=[r_psk[pb]])
                            vbi = vcount % 2
                            vcount += 1
                            S.op("act", lambda e, pb=pb, vbi=vbi: e.activation(out=vb[vbi][:], in_=psk[pb][:], func=AF.Copy),
                                 r=[r_psk[pb]], w=[r_vb[vbi]])
                            S.dma("sp", V_d[tok0 + tt * 128:tok0 + (tt + 1) * 128, head0 * 128:head0 * 128 + 512], vb[vbi][:],
                                  r=[r_vb[vbi]], w=[R_scr["V_d"]])
                if kind == "kv":
                    wf = T("wf", [128, KC, 16], BF16)
                    bfs = T("bfs", [128, 16], F32)
                    zt = [T("zt%d" % i, [128, 16], F32) for i in range(2)]
                    r_wf = Res()
                    r_zt = [Res(), Res()]
                    S.dma("pool", wf[:], w_in_v[:, :, 12288:12304], w=[r_wf])
                    S.dma("sp", bfs[:], bfrep[:, :], w=[r_wf])
                    for tt in range(8):
                        pb = u % 2
                        u += 1
                        zb = tt % 2
                        for kc in range(KC):
                            S.op("pe", lambda e, pb=pb, kc=kc, tt=tt: e.matmul(
                                psk[pb][:, 0:16], lhsT=hT[:, kc, tt * 128:(tt + 1) * 128], rhs=wf[:, kc, :],
                                start=(kc == 0), stop=(kc == KC - 1)), r=[r_wf, r_hT[tt][kc]], w=[r_psk[pb]])
                        S.op("dve", lambda e, pb=pb, zb=zb: e.tensor_tensor(out=zt[zb][:], in0=psk[pb][:, 0:16], in1=bfs[:], op=ALU.add),
                             r=[r_psk[pb], r_wf], w=[r_zt[zb]])
                        S.op("act", lambda e, zb=zb: e.activation(out=zt[zb][:], in_=zt[zb][:], func=AF.Exp, scale=-1.0), r=[r_zt[zb]], w=[r_zt[zb]])
                        S.op("act", lambda e, zb=zb: e.activation(out=zt[zb][:], in_=zt[zb][:], func=AF.Ln, bias=1.0), r=[r_zt[zb]], w=[r_zt[zb]])
                        S.dma("sp", lf_d[tok0 + tt * 128:tok0 + (tt + 1) * 128, :], zt[zb][:], r=[r_zt[zb]], w=[R_scr["lf_d"]])
                S.barrier()
        if stop_after <= 1:
            S.finish()
            return nc

        mixT_cm = nc.sbuf_tensor("mixT", [128, KC, S_Q], BF16)
        mixT = mixT_cm.__enter__()
        r_mix = [Res() for _ in range(8)]
        with ExitStack() as es:
            def T(name, shape, dt):
                return es.enter_context(nc.sbuf_tensor("a_" + name, shape, dt))

            def P(name, shape, dt):
                return es.enter_context(nc.psum_tensor("a_" + name, shape, dt))

            biasT = T("biasT", [128, 32, 72], F32)
            maskf = T("maskf", [128, 2048], F32)
            maskb = T("maskb", [128, 2048], BF16)
            pastb = T("pastb", [128, 64], F32)
            Enf = T("Enf", [8, 1024], F32)
            Enb = T("Enb", [8, 1024], BF16)
            selt = T("selt", [128, 128], F32)
            lt = T("lt", [128, 16, 16], F32)
            Lkey = T("Lkey", [128, 16, 16], F32)
            Lmid = T("Lmid", [128, 16, 16], F32)
            Lref = T("Lref", [128, 8, 16], F32)
            ltmp = T("ltmp", [128, 16, 16], F32)
            triu = T("triu", [128, 128], F32)
            onesf = T("onesf", [128, 128], F32)
            e64 = T("e64", [128, 128], F32)
            r_s = Res("attn_setup")
            psL = P("psL", [128, 512], F32)
            r_psL = Res()
            S.dma("sp", biasT[:, 0:16, :].rearrange("p h c -> p (h c)"), albias[:, :], w=[r_s])
            S.dma("sp", maskf[:], maskd[:, :], w=[r_s])
            S.dma("sp", pastb[:], pastd[:, :], w=[r_s])
            S.dma("sp", Enf[:], End[:, :], w=[r_s])
            S.dma("sp", selt[:], seld[:, :], w=[r_s])
            S.dma("sp", lt[:], lf_d.rearrange("(j p) h -> p j h", p=128), r=[R_scr["lf_d"]], w=[r_s])
            S.op("dve", lambda e: e.tensor_copy(out=maskb[:], in_=maskf[:]), r=[r_s], w=[r_s])
            S.op("dve", lambda e: e.tensor_copy(out=Enb[:], in_=Enf[:]), r=[r_s], w=[r_s])
            S.op("pool", lambda e: e.memset(onesf[:], 1.0), w=[r_s])
            S.op("pool", lambda e: e.memset(triu[:], 1.0), w=[r_s])
            S.op("pool", lambda e: e.affine_select(out=triu[:], in_=triu[:], pattern=[[1, 128]], compare_op=ALU.is_ge,
                                                   fill=0.0, base=0, channel_multiplier=-1), r=[r_s], w=[r_s])
            S.op("pool", lambda e: e.memset(e64[:], 0.0), w=[r_s])
            S.op("pool", lambda e: e.affine_select(out=e64[:], in_=e64[:], pattern=[[0, 128]], compare_op=ALU.not_equal,
                                                   fill=1.0, base=-64, channel_multiplier=1), r=[r_s], w=[r_s])
            for j in range(16):
                for i in range(j + 1):
                    S.op("pe", lambda e, i=i, j=j: e.matmul(psL[:, 0:16], lhsT=(triu[:] if i == j else onesf[:]), rhs=lt[:, i, :],
                                                            start=(i == 0), stop=(i == j)), r=[r_s], w=[r_psL])
                S.op("dve", lambda e, j=j: e.tensor_copy(out=Lkey[:, j, :], in_=psL[:, 0:16]), r=[r_psL], w=[r_s])
            S.op("pe", lambda e: e.matmul(psL[:, 0:256], lhsT=e64[:], rhs=Lkey[:].rearrange("p j h -> p (j h)"), start=True, stop=True),
                 r=[r_s], w=[r_psL])
            S.op("dve", lambda e: e.tensor_copy(out=Lmid[:].rearrange("p j h -> p (j h)"), in_=psL[:, 0:256]), r=[r_psL], w=[r_s])
            for r in range(8):
                S.op("dve", lambda e, r=r: e.tensor_tensor(
                    out=ltmp[:], in0=Lmid[:], in1=selt[:, r * 16:(r + 1) * 16].rearrange("p (g o) -> p g o", o=1).to_broadcast([128, 16, 16]),
                    op=ALU.mult), r=[r_s], w=[r_s])
                S.op("dve", lambda e, r=r: e.tensor_reduce(out=Lref[:, r, :], in_=ltmp[:].rearrange("p g h -> p h g"), axis=AX.X, op=ALU.add),
                     r=[r_s], w=[r_s])
            for h in range(16):
                for r in range(8):
                    nk = 2 * r + 2
                    S.op("dve", lambda e, h=h, r=r, nk=nk: e.tensor_scalar(
                        out=biasT[:, 16 + h, OFF[r]:OFF[r] + nk], in0=Lkey[:, 0:nk, h], scalar1=Lref[:, r, h:h + 1], scalar2=None,
                        op0=ALU.subtract), r=[r_s], w=[r_s])

            wa2 = [T("wa2_%d" % i, [128, KC, 512], BF16) for i in range(2)]
            brow2 = [T("brow2_%d" % i, [1, 512], F32) for i in range(2)]
            orow2 = [T("orow2_%d" % i, [1, 512], F32) for i in range(2)]
            r_wa2 = [Res(), Res()]
            r_brow2 = [Res(), Res()]
            r_orow2 = [Res(), Res()]
            wload(wa2[0], w_ada_v, 16 * 512, 512, r_wa2[0])
            KT = [T("KT%d" % i, [128, S_ALL], BF16) for i in range(2)]
            QT = [T("QT%d" % i, [128, S_Q], BF16) for i in range(2)]
            Vh = [T("Vh%d" % i, [128, 16, 128], BF16) for i in range(2)]
            r_KT = [Res(), Res()]
            r_QT = [Res(), Res()]
            r_Vh = [Res(), Res()]
            PT = [T("PT%d" % i, [128, 1024], BF16) for i in range(2)]
            r_PT = [[Res() for _ in range(8)] for _ in range(2)] if os.environ.get('FINE_PT', '1') == '1' else [[Res()] * 8 for _ in range(2)]
            psS = [P("psS%d" % i, [128, 1024], F32) for i in range(2)]
            r_psS = [[Res() for _ in range(8)] for _ in range(2)] if os.environ.get('FINE_ATT', '0') == '1' else [[Res()] * 8 for _ in range(2)]
            psOZ = [P("psOZ%d" % i, [128, 256], F32) for i in range(2)]
            r_psOZ = [Res(), Res()]
            psG = P("psG", [128, 128], F32)
            r_psG = Res()
            kmf = T("kmf", [128, 8], F32)
            kmb = T("kmb", [128, 8], BF16)
            gsb = T("gsb", [128, 8], F32)
            g8 = T("g8", [128, 8], F32)
            selb = T("selb", [128, 8], F32)
            selT = [T("selT%d" % i, [8, 128], BF16) for i in range(2)]
            r_g = Res()
            r_selT = [Res(), Res()]
            rec = T("rec", [128, 128], F32)
            r_rec = Res()
            V_v = V_d.rearrange("(j p) c -> p j c", p=128)

            def load_head(H, b):
                S.dma("sp", KT[b][:], KT_d[H], r=[R_scr["KT_d"]], w=[r_KT[b]])
                S.dma("sp", QT[b][:], QT_d[H], r=[R_scr["QT_d"]], w=[r_QT[b]])
                S.dma("sp", Vh[b][:], V_v[:, :, H * 128:(H + 1) * 128], r=[R_scr["V_d"]], w=[r_Vh[b]])

            scale = 128.0 ** -0.5
            units = []
            for H in range(32):
                for r in range(8):
                    nk = 2 * r + 2
                    for c0 in range(0, nk, 8):
                        units.append((H, r, c0, list(range(c0, min(c0 + 8, nk)))))
            gstate = {"gcount": 0, "sb": {}}

            def emit_scores(ui):
                H, r, c0, tiles = units[ui]
                b = H % 2
                ub = ui % 2
                nk = 2 * r + 2
                moba = H < 16
                gate = moba and r >= 4
                if r == 0 and c0 == 0:
                    g_ = 16 + H
                    gb = g_ % 2
                    if g_ + 1 < 48:
                        wload(wa2[1 - gb], w_ada_v, (g_ + 1) * 512, 512, r_wa2[1 - gb])
                    ada_group(g_, wa2[gb], brow2[gb], orow2[gb], psL[0:1, :], r_wa2[gb], r_brow2[gb], r_orow2[gb], r_psL)
                    if moba:
                        S.op("dve", lambda e: e.tensor_reduce(out=kmf[:], in_=KT[b][:].rearrange("d (n s) -> d n s", s=256), axis=AX.X, op=ALU.add),
                             r=[r_KT[b]], w=[r_g])
                        S.op("dve", lambda e: e.tensor_scalar(out=kmb[:], in0=kmf[:], scalar1=1.0 / 256, scalar2=None, op0=ALU.mult), r=[r_g], w=[r_g])
                if gate and c0 == 0:
                    sb = gstate["gcount"] % 2
                    gstate["gcount"] += 1
                    gstate["sb"][(H, r)] = sb
                    S.op("pe", lambda e: e.matmul(psG[:, 0:8], lhsT=QT[b][:, r * 128:(r + 1) * 128], rhs=kmb[:, 0:8], start=True, stop=True),
                         r=[r_QT[b], r_g], w=[r_psG])
                    S.op("dve", lambda e: e.tensor_tensor(out=gsb[:], in0=psG[:, 0:8], in1=pastb[:, r * 8:(r + 1) * 8], op=ALU.add),
                         r=[r_psG, r_s], w=[r_g])
                    S.op("dve", lambda e: e.max(out=g8[:], in_=gsb[:]), r=[r_g], w=[r_g])
                    S.op("dve", lambda e: e.tensor_scalar(out=selb[:], in0=gsb[:], scalar1=g8[:, 2:3], scalar2=NEG, op0=ALU.is_lt, op1=ALU.mult),
                         r=[r_g], w=[r_g])
                    S.op("pe", lambda e: e.matmul(psG[0:8, 0:128], lhsT=selb[:], rhs=identf[:], start=True, stop=True),
                         r=[r_g, r_const], w=[r_psG])
                    S.op("act", lambda e: e.activation(out=selT[sb][:], in_=psG[0:8, 0:128], func=AF.Copy), r=[r_psG], w=[r_selT[sb]])
                sb = gstate["sb"].get((H, r), 0)
                for j in tiles:
                    o = (j - c0) * 128
                    is_mask = j >= nk - 2
                    is_sel = gate and not is_mask
                    extra = is_mask or is_sel
                    S.op("pe", lambda e: e.matmul(
                        psS[ub][:, o:o + 128], lhsT=KT[b][:, j * 128:(j + 1) * 128], rhs=QT[b][:, r * 128:(r + 1) * 128],
                        start=True, stop=(not extra)), r=[r_KT[b], r_QT[b]], w=[r_psS[ub][j - c0]])
                    if is_mask:
                        mi = r * 2 + (j - (nk - 2))
                        S.op("pe", lambda e: e.matmul(
                            psS[ub][:, o:o + 128], lhsT=identb[:], rhs=maskb[:, mi * 128:(mi + 1) * 128], start=False, stop=True),
                             r=[r_s, r_const], w=[r_psS[ub][j - c0]])
                    elif is_sel:
                        n = j // 2
                        S.op("pe", lambda e: e.matmul(
                            psS[ub][:, o:o + 128], lhsT=Enb[:, n * 128:(n + 1) * 128], rhs=selT[sb][:], start=False, stop=True),
                             r=[r_s, r_selT[sb]], w=[r_psS[ub][j - c0]])

            def emit_exps(ui):
                H, r, c0, tiles = units[ui]
                ub = ui % 2
                for j in tiles:
                    o = (j - c0) * 128
                    S.op("act", lambda e: e.activation(
                        out=PT[ub][:, o:o + 128], in_=psS[ub][:, o:o + 128], func=AF.Exp,
                        bias=biasT[:, H, OFF[r] + j:OFF[r] + j + 1], scale=scale), r=[r_psS[ub][j - c0], r_s], w=[r_PT[ub][j - c0]])

            def emit_pv(ui):
                H, r, c0, tiles = units[ui]
                b = H % 2
                ub = ui % 2
                nk = 2 * r + 2
                ob = (H * 8 + r) % 2
                for j in tiles:
                    o = (j - c0) * 128
                    S.op("pe", lambda e: e.matmul(
                        psOZ[ob][:, 0:128], lhsT=Vh[b][:, j, :], rhs=PT[ub][:, o:o + 128], start=(j == 0), stop=(j == nk - 1)),
                         r=[r_Vh[b], r_PT[ub][j - c0]], w=[r_psOZ[ob]])
                    S.op("pe", lambda e: e.matmul(
                        psOZ[ob][:, 128:256], lhsT=onesb[:], rhs=PT[ub][:, o:o + 128], start=False, stop=(j == nk - 1), skip_group_check=True),
                         r=[r_const, r_PT[ub][j - c0]], w=[r_psOZ[ob]])
                if tiles[-1] == nk - 1:
                    S.op("dve", lambda e: e.reciprocal(out=rec[:], in_=psOZ[ob][:, 128:256]), r=[r_psOZ[ob]], w=[r_rec])
                    S.op("dve", lambda e: e.tensor_tensor(out=mixT[:, H, r * 128:(r + 1) * 128], in0=psOZ[ob][:, 0:128], in1=rec[:], op=ALU.mult),
                         r=[r_psOZ[ob], r_rec], w=[r_mix[r]])
                    if r == 7 and H + 2 < 32:
                        load_head(H + 2, b)

            load_head(0, 0)
            load_head(1, 1)
            emit_scores(0)
            for ui in range(len(units)):
                if ui + 1 < len(units):
                    emit_scores(ui + 1)
                emit_exps(ui)
                emit_pv(ui)
            S.barrier()
        build_modT(64, 192, "b")
        S.op("dve", lambda e: e.scalar_tensor_tensor(out=A2[:], in0=modT[:, 4 * KC:5 * KC], scalar=1.0, in1=gn[:, KC:2 * KC],
                                                     op0=ALU.add, op1=ALU.mult), r=[r_mod, r_const], w=[r_mod])
        S.barrier()
        if stop_after <= 2:
            S.finish()
            mixT_cm.__exit__(None, None, None)
            return nc

        with ExitStack() as es:
            def T(name, shape, dt):
                return es.enter_context(nc.sbuf_tensor("o_" + name, shape, dt))

            def P(name, shape, dt):
                return es.enter_context(nc.psum_tensor("o_" + name, shape, dt))

            g1rep = T("g1rep", [128, D], F32)
            r_g1 = Res()
            for i in range(4):
                S.dma("sp", g1rep[:, i * 1024:(i + 1) * 1024],
                      mod_d[64 + i * 8:64 + (i + 1) * 8, :].rearrange("(o j) p -> o (j p)", o=1).partition_broadcast(128),
                      r=[R_scr["mod_d"]], w=[r_g1])
            wo = [T("wo%d" % i, [128, KC, 512], BF16) for i in range(2)]
            r_wo = [Res(), Res()]
            pso = [P("pso%d" % i, [128, 512], F32) for i in range(2)]
            r_pso = [Res(), Res()]
            xs = [T("xs%d" % i, [128, 512], F32) for i in range(2)]
            t1 = [T("t1%d" % i, [128, 512], F32) for i in range(2)]
            r_xs = [Res(), Res()]
            r_t1 = [Res(), Res()]
            wload(wo[0], w_out_v, 0, 512, r_wo[0])
            u = 0
            for ng in range(8):
                b = ng % 2
                if ng + 1 < 8:
                    wload(wo[1 - b], w_out_v, (ng + 1) * 512, 512, r_wo[1 - b])
                for tt in range(8):
                    pb = u % 2
                    u += 1
                    S.dma("sp", xs[pb][:], xq[tt * 128:(tt + 1) * 128, ng * 512:(ng + 1) * 512], w=[r_xs[pb]])
                    for kc in range(KC):
                        S.op("pe", lambda e, pb=pb, kc=kc, tt=tt, b=b: e.matmul(
                            pso[pb][:], lhsT=mixT[:, kc, tt * 128:(tt + 1) * 128], rhs=wo[b][:, kc, :], start=(kc == 0), stop=(kc == KC - 1)),
                             r=[r_wo[b], r_mix[tt]], w=[r_pso[pb]])
                    S.op("dve", lambda e, pb=pb, ng=ng: e.tensor_tensor(out=t1[pb][:], in0=pso[pb][:], in1=g1rep[:, ng * 512:(ng + 1) * 512], op=ALU.mult),
                         r=[r_pso[pb], r_g1], w=[r_t1[pb]])
                    S.op("dve", lambda e, pb=pb: e.tensor_tensor(out=t1[pb][:], in0=t1[pb][:], in1=xs[pb][:], op=ALU.add),
                         r=[r_t1[pb], r_xs[pb]], w=[r_t1[pb]])
                    S.dma("sp", x1_d[tt * 128:(tt + 1) * 128, ng * 512:(ng + 1) * 512], t1[pb][:], r=[r_t1[pb]], w=[R_scr["x1_d"]])
            S.barrier()
        mixT_cm.__exit__(None, None, None)
        if stop_after <= 3:
            S.finish()
            return nc

        with ExitStack() as es:
            def T(name, shape, dt):
                return es.enter_context(nc.sbuf_tensor("e_" + name, shape, dt))

            def P(name, shape, dt):
                return es.enter_context(nc.psum_tensor("e_" + name, shape, dt))

            h2T = T("h2T", [128, KC, S_Q], BF16)
            r_h2 = [[Res() for _ in range(KC)] for _ in range(8)]
            with ExitStack() as es2:
                build_hT(es2, h2T, r_h2, x1_d, 0, A2, modT[:, 3 * KC:4 * KC], "e_")
            RC = T("RC", [128, 8, 8, 2, 128], F32)
            tau2 = T("tau2", [128, 64], F32)
            r_RC = [[Res() for _ in range(8)] for _ in range(8)]
            with ExitStack() as es2:
                def T2(name, shape, dt):
                    return es2.enter_context(nc.sbuf_tensor("e7_" + name, shape, dt))

                def P2(name, shape, dt):
                    return es2.enter_context(nc.psum_tensor("e7_" + name, shape, dt))

                skf = T2("skf", [128, 2048], F32)
                skb = T2("skb", [128, 2048], BF16)
                r_sk = Res()
                S.dma("sp", skf[:], skT[:, :], w=[r_sk])
                S.op("dve", lambda e: e.tensor_copy(out=skb[:], in_=skf[:]), r=[r_sk], w=[r_sk])
                wq = [T2("wq%d" % i, [128, KC, 256], BF16) for i in range(2)]
                r_wq = [Res(), Res()]
                psq = [P2("psq%d" % i, [128, 512], F32) for i in range(2)]
                r_psq = [Res(), Res()]
                qTb = [T2("qTb%d" % i, [128, 1024], BF16) for i in range(2)]
                r_qT = [Res(), Res()]
                NCH = 3
                pssc = [[P2("pssc%d_%d" % (i, c), [128, 128], F32) for c in range(2)] for i in range(NCH)]
                sets = []
                for i in range(NCH):
                    sets.append(dict(
                        scs=[T2("scs%d_%d" % (i, c), [128, 128], F32) for c in range(2)],
                        m8=T2("m8_%d" % i, [128, 8, 8], F32), rv=T2("rv%d" % i, [128, 2, 16], F32),
                        tmpR=T2("tmpR%d" % i, [128, 128], F32), G=T2("G%d" % i, [128, 256], F32), G2=T2("G2_%d" % i, [128, 256], F32),
                        zz=T2("zz%d" % i, [128, 8], F32), pssc=pssc[i],
                        r_pssc=[Res(), Res()], r_scs=[Res(), Res()], r_m=[Res(), Res()], r_rv=[Res(), Res()], r_t=[Res()] * 2, r_g=Res()))

                def chain(tt, h, st):
                    m8, rv, tmpR, G, G2, zz = st["m8"], st["rv"], st["tmpR"], st["G"], st["G2"], st["zz"]
                    rr = r_RC[tt][h]
                    for c in range(2):
                        hc = h * 2 + c
                        scs = st["scs"][c]
                        Rc = RC[:, tt, h, c, :]
                        r_ps, r_sc, r_m, r_rv, r_t = st["r_pssc"][c], st["r_scs"][c], st["r_m"][c], st["r_rv"][c], st["r_t"][c]
                        S.op("pe", lambda e: e.matmul(st["pssc"][c][:], lhsT=qTb[c][:, tt * 128:(tt + 1) * 128], rhs=skb[:, hc * 128:(hc + 1) * 128],
                                                      start=True, stop=True), r=[r_qT[c], r_sk], w=[r_ps])
                        yield
                        S.op("act", lambda e: e.activation(out=scs[:], in_=st["pssc"][c][:], func=AF.Copy), r=[r_ps], w=[r_sc])
                        yield
                        S.op("dve", lambda e: e.max(out=m8[:, c, :], in_=scs[:]), r=[r_sc], w=[r_m])
                        yield
                        S.op("dve", lambda e: e.tensor_scalar(out=m8[:, 4 + c, 0:1], in0=m8[:, c, 0:1], scalar1=-1.0, scalar2=None, op0=ALU.mult),
                             r=[r_m], w=[r_m])
                        yield
                        S.op("act", lambda e: e.activation(out=Rc, in_=scs[:], func=AF.Exp, bias=m8[:, 4 + c, 0:1], scale=1.0), r=[r_sc, r_m], w=[rr])
                        yield
                        S.op("dve", lambda e: e.max(out=rv[:, c, 0:8], in_=Rc), r=[rr], w=[r_rv])
                        yield
                        S.op("dve", lambda e: e.match_replace(out=tmpR[:], in_to_replace=rv[:, c, 0:8], in_values=Rc, imm_value=-1.0), r=[rr, r_rv], w=[r_t])
                        yield
                        S.op("dve", lambda e: e.max(out=rv[:, c, 8:16], in_=tmpR[:]), r=[r_t], w=[r_rv])
                        yield
                        S.op("dve", lambda e: e.scalar_tensor_tensor(out=Rc, in0=Rc, scalar=rv[:, c, 15:16], in1=Rc, op0=ALU.is_ge, op1=ALU.mult),
                             r=[rr, r_rv], w=[rr])
                        yield
                    r_g = st["r_g"]
                    S.op("dve", lambda e: e.tensor_tensor(
                        out=G[:].rearrange("p (a b) -> p a b", a=16),
                        in0=rv[:, 0, :].rearrange("p (a o) -> p a o", o=1).to_broadcast([128, 16, 16]),
                        in1=rv[:, 1, :].rearrange("p (o b) -> p o b", o=1).to_broadcast([128, 16, 16]), op=ALU.mult),
                         r=[st["r_rv"][0], st["r_rv"][1]], w=[r_g])
                    yield
                    S.op("dve", lambda e: e.max(out=m8[:, 2, :], in_=G[:]), r=[r_g], w=[r_g])
                    yield
                    S.op("dve", lambda e: e.match_replace(out=G2[:], in_to_replace=m8[:, 2, :], in_values=G[:], imm_value=-1.0), r=[r_g], w=[r_g])
                    yield
                    S.op("dve", lambda e: e.max(out=m8[:, 3, :], in_=G2[:]), r=[r_g], w=[r_g])
                    yield
                    S.op("dve", lambda e: e.memset(zz[:, 0:1], 0.0), w=[r_g])
                    yield
                    S.op("dve", lambda e: e.scalar_tensor_tensor(out=G2[:], in0=G[:], scalar=m8[:, 3, 7:8], in1=G[:], op0=ALU.is_ge, op1=ALU.mult,
                                                                 accum_out=zz[:, 0:1]), r=[r_g], w=[r_g])
                    yield
                    S.op("dve", lambda e: e.reciprocal(out=zz[:, 1:2], in_=zz[:, 0:1]), r=[r_g], w=[r_g])
                    yield
                    S.op("dve", lambda e: e.tensor_scalar(out=RC[:, tt, h, 0, :], in0=RC[:, tt, h, 0, :], scalar1=zz[:, 1:2], scalar2=None, op0=ALU.mult),
                         r=[rr, r_g], w=[rr])
                    yield
                    S.op("dve", lambda e: e.tensor_scalar(out=tau2[:, tt * 8 + h:tt * 8 + h + 1], in0=m8[:, 3, 7:8], scalar1=zz[:, 1:2],
                                                          scalar2=0.99999, op0=ALU.mult, op1=ALU.mult), r=[r_g], w=[rr])
                    yield

                wload(wq[0], w_pq_v, 0, 256, r_wq[0])
                u = 0
                for h in range(8):
                    b = h % 2
                    if h + 1 < 8:
                        wload(wq[1 - b], w_pq_v, (h + 1) * 256, 256, r_wq[1 - b])
                    for c in range(2):
                        for tg in range(2):
                            pb = u % 2
                            u += 1
                            for kc in range(KC):
                                S.op("pe", lambda e, pb=pb, kc=kc, c=c, tg=tg, b=b: e.matmul(
                                    psq[pb][:], lhsT=wq[b][:, kc, c * 128:(c + 1) * 128], rhs=h2T[:, kc, tg * 512:(tg + 1) * 512],
                                    start=(kc == 0), stop=(kc == KC - 1)), r=[r_wq[b]] + [r_h2[t_][kc] for t_ in range(tg * 4, tg * 4 + 4)], w=[r_psq[pb]])
                            S.op("act", lambda e, pb=pb, c=c, tg=tg: e.activation(out=qTb[c][:, tg * 512:(tg + 1) * 512], in_=psq[pb][:], func=AF.Copy),
                                 r=[r_psq[pb]], w=[r_qT[c]])
                    for t0 in range(0, 8, NCH):
                        gens = [chain(t0 + i, h, sets[i]) for i in range(NCH) if t0 + i < 8]
                        alive = list(gens)
                        while alive:
                            nxt = []
                            for g in alive:
                                try:
                                    next(g)
                                    nxt.append(g)
                                except StopIteration:
                                    pass
                            alive = nxt
                S.barrier()
            with ExitStack() as es2:
                def T2(name, shape, dt):
                    return es2.enter_context(nc.sbuf_tensor("e8_" + name, shape, dt))

                def P2(name, shape, dt):
                    return es2.enter_context(nc.psum_tensor("e8_" + name, shape, dt))

                NB = 8
                ub_ = [T2("ub%d" % i, [128, KC, 256], BF16) for i in range(2)]
                r_ub = [Res(), Res()]
                psA = [P2("psA%d" % i, [128, 512], F32) for i in range(2)]
                r_psA = [Res(), Res()]
                psW = [P2("psW%d" % i, [128, 512], F32) for i in range(2)]
                r_psW = [[Res() for _ in range(4)] for _ in range(2)]
                AgT = [T2("AgT%d" % i, [128, 512], F32) for i in range(2)]
                r_Ag = [Res(), Res()]
                Prb = [T2("Prb%d" % i, [128, 128], F32) for i in range(NB)]
                r_Pr = [Res() for _ in range(NB)]
                Mb = [T2("Mb%d" % i, [128, 128], BF16) for i in range(NB)]
                r_M = [Res() for _ in range(NB)]
                WgTb = [T2("WgTb%d" % i, [128, 512], BF16) for i in range(2)]
                r_WgT = [Res(), Res()]
                items = [(eg, tg, k) for eg in range(64) for tg in range(2) for k in range(2)]
                NI = len(items)

                def emit_A(n, kc):
                    eg, tg, k = items[n]
                    b = eg % 2
                    pa = n % 2
                    if kc == 0 and tg == 0 and k == 0 and eg + 1 < 64:
                        wload(ub_[1 - b], uT_v, (eg + 1) * 256, 256, r_ub[1 - b])
                    S.op("pe", lambda e: e.matmul(
                        psA[pa][:], lhsT=ub_[b][:, kc, k * 128:(k + 1) * 128], rhs=h2T[:, kc, tg * 512:(tg + 1) * 512],
                        start=(kc == 0), stop=(kc == KC - 1)),
                         r=[r_ub[b]] + [r_h2[t_][kc] for t_ in range(tg * 4, tg * 4 + 4)], w=[r_psA[pa]])

                def emit_gelu(n):
                    pa = n % 2
                    S.op("act", lambda e: e.activation(out=AgT[pa][:], in_=psA[pa][:], func=AF.Gelu), r=[r_psA[pa]], w=[r_Ag[pa]])

                wload(ub_[0], uT_v, 0, 256, r_ub[0])
                for kc in range(KC):
                    emit_A(0, kc)
                emit_gelu(0)
                pc = 0
                for n in range(NI):
                    eg, tg, k = items[n]
                    i = eg * 2 + k
                    pa = n % 2
                    for q in range(32):
                        tl, h = q // 8, q % 8
                        tt = tg * 4 + tl
                        pi = pc % NB
                        pc += 1
                        S.op("act", lambda e, pi=pi, tt=tt, h=h, i=i: e.mul(out=Prb[pi][:], in_=RC[:, tt, h, 1, :], mul=RC[:, tt, h, 0, i:i + 1]),
                             r=[r_RC[tt][h]], w=[r_Pr[pi]])
                        S.op("dve", lambda e, pi=pi, tt=tt, h=h: e.scalar_tensor_tensor(
                            out=Mb[pi][:], in0=Prb[pi][:], scalar=tau2[:, tt * 8 + h:tt * 8 + h + 1], in1=Prb[pi][:],
                            op0=ALU.is_ge, op1=ALU.mult), r=[r_Pr[pi], r_RC[tt][h]], w=[r_M[pi]])
                        if n + 1 < NI:
                            emit_A(n + 1, q)
                        S.op("pe", lambda e, pi=pi, pa=pa, tl=tl, h=h: e.matmul(
                            psW[pa][:, tl * 128:(tl + 1) * 128], lhsT=Mb[pi][:], rhs=identb[:], start=(h == 0), stop=(h == 7)),
                             r=[r_M[pi], r_const], w=[r_psW[pa][tl]])
                    if n + 1 < NI:
                        emit_gelu(n + 1)
                    S.op("dve", lambda e, pa=pa: e.tensor_tensor(out=WgTb[pa][:], in0=psW[pa][:], in1=AgT[pa][:], op=ALU.mult),
                         r=r_psW[pa] + [r_Ag[pa]], w=[r_WgT[pa]])
                    S.dma("sp", WgT_d[i * 128:(i + 1) * 128, tg * 512:(tg + 1) * 512], WgTb[pa][:], r=[r_WgT[pa]], w=[R_scr["WgT_d"]])
            S.barrier()
        if stop_after <= 4:
            S.finish()
            return nc

        with ExitStack() as es:
            def T(name, shape, dt):
                return es.enter_context(nc.sbuf_tensor("f_" + name, shape, dt))

            def P(name, shape, dt):
                return es.enter_context(nc.psum_tensor("f_" + name, shape, dt))

            g2rep = T("g2rep", [128, D], F32)
            r_g2 = Res()
            for i in range(4):
                S.dma("sp", g2rep[:, i * 1024:(i + 1) * 1024],
                      mod_d[160 + i * 8:160 + (i + 1) * 8, :].rearrange("(o j) p -> o (j p)", o=1).partition_broadcast(128),
                      r=[R_scr["mod_d"]], w=[r_g2])
            vbuf = [T("vbuf%d" % i, [128, 8, 512], BF16) for i in range(2)]
            wbuf = [T("wbuf%d" % i, [128, 8, S_Q], BF16) for i in range(2)]
            r_vbuf = [Res(), Res()]
            r_wbuf = [Res(), Res()]
            psY = [P("psY%d" % i, [128, 512], F32) for i in range(8)]
            r_psY = [Res() for _ in range(8)]
            xs = [T("xs%d" % i, [128, 512], F32) for i in range(2)]
            t1 = [T("t1%d" % i, [128, 512], F32) for i in range(2)]
            r_xs = [Res(), Res()]
            r_t1 = [Res(), Res()]
            pv_v = pv.rearrange("(et p) d -> p et d", p=128)
            WgT_v = WgT_d.rearrange("(et p) t -> p et t", p=128)

            def load_batch(dg, eb, b):
                for hlf in range(2):
                    S.dma("pool", vbuf[b][:, hlf * 4:(hlf + 1) * 4, :], pv_v[:, eb * 8 + hlf * 4:eb * 8 + (hlf + 1) * 4, dg * 512:(dg + 1) * 512], w=[r_vbuf[b]])
                for hlf in range(2):
                    S.dma("sp", wbuf[b][:, hlf * 4:(hlf + 1) * 4, :], WgT_v[:, eb * 8 + hlf * 4:eb * 8 + (hlf + 1) * 4, :], r=[R_scr["WgT_d"]], w=[r_wbuf[b]])

            seq = [(dg, eb) for dg in range(8) for eb in range(16)]
            load_batch(0, 0, 0)
            u = 0
            for si, (dg, eb) in enumerate(seq):
                b = si % 2
                if si + 1 < len(seq):
                    load_batch(seq[si + 1][0], seq[si + 1][1], 1 - b)
                for et in range(8):
                    for tt in range(8):
                        S.op("pe", lambda e, b=b, et=et, tt=tt, eb=eb: e.matmul(
                            psY[tt][:], lhsT=wbuf[b][:, et, tt * 128:(tt + 1) * 128], rhs=vbuf[b][:, et, :],
                            start=(eb == 0 and et == 0), stop=(eb == 15 and et == 7)), r=[r_vbuf[b], r_wbuf[b]], w=[r_psY[tt]])
                if eb == 15:
                    for tt in range(8):
                        pb = u % 2
                        u += 1
                        S.dma("sp", xs[pb][:], x1_d[tt * 128:(tt + 1) * 128, dg * 512:(dg + 1) * 512], r=[R_scr["x1_d"]], w=[r_xs[pb]])
                        S.op("dve", lambda e, pb=pb, tt=tt, dg=dg: e.tensor_tensor(out=t1[pb][:], in0=psY[tt][:], in1=g2rep[:, dg * 512:(dg + 1) * 512], op=ALU.mult),
                             r=[r_psY[tt], r_g2], w=[r_t1[pb]])
                        S.op("dve", lambda e, pb=pb: e.tensor_tensor(out=t1[pb][:], in0=t1[pb][:], in1=xs[pb][:], op=ALU.add),
                             r=[r_t1[pb], r_xs[pb]], w=[r_t1[pb]])
                        S.dma("sp", out[tt * 128:(tt + 1) * 128, dg * 512:(dg + 1) * 512], t1[pb][:], r=[r_t1[pb]], w=[R_scr["out"]])
        S.finish()
    return nc


def host_inputs(x, c, w_ada, b_ada, norm1_g, w_in, b_f, q_norm_moba, k_norm_moba, q_norm_fox, k_norm_fox,
                w_out, norm2_g, w_pq, peer_sub_keys, peer_u, peer_v, cores=None):
    f = np.float32
    x = np.asarray(x, f)
    c = np.asarray(c, f)
    shared = {
        "w_ada": np.ascontiguousarray(np.asarray(w_ada, f)[0]),
        "b_ada": np.ascontiguousarray(np.asarray(b_ada, f)[0][None, :]),
        "n1g": np.ascontiguousarray(np.asarray(norm1_g, f)[0].reshape(KC, 128).T),
        "n2g": np.ascontiguousarray(np.asarray(norm2_g, f)[0].reshape(KC, 128).T),
        "w_in": np.ascontiguousarray(np.asarray(w_in, f)[0]),
        "bfrep": np.ascontiguousarray(np.broadcast_to(np.asarray(b_f, f)[0][None, :], (128, 16))),
        "gains": np.ascontiguousarray(np.stack([np.asarray(q_norm_moba, f)[0], np.asarray(k_norm_moba, f)[0],
                                                np.asarray(q_norm_fox, f)[0], np.asarray(k_norm_fox, f)[0]], axis=1)),
        "w_out": np.ascontiguousarray(np.asarray(w_out, f)[0]),
        "w_pq": np.ascontiguousarray(np.asarray(w_pq, f)[0]),
        "skT": np.ascontiguousarray(np.asarray(peer_sub_keys, f)[0].transpose(3, 0, 1, 2).reshape(128, 2048)),
        "uT": np.ascontiguousarray(np.asarray(peer_u, f)[0].T),
        "pv": np.ascontiguousarray(np.asarray(peer_v, f)[0]),
    }
    past = np.zeros((128, 8, 8), f)
    for r in range(8):
        past[:, r, r:] = -1e30
    shared["pastb"] = past.reshape(128, 64)
    En = np.zeros((8, 8, 128), f)
    for n in range(8):
        En[n, n, :] = 1.0
    shared["En"] = En.reshape(8, 1024)
    slopes = 2.0 ** (-8.0 * np.arange(1, 17, dtype=np.float64) / 16)
    p = np.arange(128)
    tri = np.where(p[:, None] <= p[None, :], 0.0, NEG).astype(f)
    in_maps = []
    for cid in (range(8) if cores is None else cores):
        b, hf = cid // 2, cid % 2
        gt = [gtile(r, hf) for r in range(8)]
        xb = x[b]
        xq = np.concatenate([xb[g * 128:(g + 1) * 128] for g in gt], axis=0)
        al = np.zeros((128, 16, 72), np.float64)
        sel = np.zeros((128, 8, 16), f)
        mk = np.zeros((128, 16, 128), f)
        for r in range(8):
            g = gt[r]
            tmid = 128 * g + 64
            sel[:, r, g] = 1.0
            for j in range(2 * r + 2):
                al[:, :, OFF[r] + j] = slopes[None, :] * (128 * j + p[:, None] - tmid)
            if g == 2 * r:
                mk[:, 2 * r, :] = tri
                mk[:, 2 * r + 1, :] = NEG
            else:
                mk[:, 2 * r, :] = 0.0
                mk[:, 2 * r + 1, :] = tri
        m = dict(shared)
        m["xall"] = np.ascontiguousarray(xb)
        m["xq"] = np.ascontiguousarray(xq)
        m["cT"] = np.ascontiguousarray(c[b].reshape(KC, 128).T)
        m["albias"] = np.ascontiguousarray(al.astype(f).reshape(128, 16 * 72))
        m["sel"] = np.ascontiguousarray(sel.reshape(128, 128))
        m["maskb"] = np.ascontiguousarray(mk.reshape(128, 2048))
        in_maps.append(m)
    return in_maps


def kernel(**inputs):
    in_maps = host_inputs(**inputs)
    nc = build()
    res = run_bass_kernel_spmd(nc, in_maps, core_ids=list(range(8)))
    B, Sq = 4, 2048
    outf = np.zeros((B, Sq, D), np.float32)
    for cid in range(8):
        b, hf = cid // 2, cid % 2
        o = np.asarray(res.results[cid]["out"])
        for r in range(8):
            g = gtile(r, hf)
            outf[b, g * 128:(g + 1) * 128] = o[r * 128:(r + 1) * 128]
    return outf
```

```python
import numpy as np
import os
from contextlib import ExitStack
import concourse.bass as bass
import concourse.mybir as mybir
from concourse.bass_utils import run_bass_kernel_spmd

F32 = mybir.dt.float32
BF16 = mybir.dt.bfloat16
AF = mybir.ActivationFunctionType
ALU = mybir.AluOpType
AX = mybir.AxisListType

D = 4096
KC = 32
S_ALL = 2048
S_Q = 1024
NEG = -30000.0
EPS = 1e-6
OFF = [r * (r + 1) for r in range(9)]


class Res:
    __slots__ = ("name", "lw", "rd")

    def __init__(self, name=""):
        self.name = name
        self.lw = None
        self.rd = {}


class Sched:
    def __init__(self, nc, ndma=8):
        self.nc = nc
        self.eng = {"pe": nc.tensor, "act": nc.scalar, "dve": nc.vector, "pool": nc.gpsimd, "sp": nc.sync}
        self.sem = {}
        self.cnt = {}
        self.seen = {e: {} for e in self.eng}
        for e in ("pe", "act", "dve", "pool"):
            self.sem[e] = nc.alloc_semaphore(name="c_" + e)
            self.cnt[e] = 0
        self.dpool = {}
        self.dnext = {}
        for q in ("sp", "pool"):
            self.dpool[q] = []
            for i in range(ndma):
                k = "d_%s%d" % (q, i)
                self.sem[k] = nc.alloc_semaphore(name=k)
                self.cnt[k] = 0
                self.dpool[q].append(k)
            self.dnext[q] = 0
        self.nins = 0

    def _wait(self, e, key, val):
        if val <= 0 or self.seen[e].get(key, 0) >= val:
            return
        self.eng[e].wait_ge(self.sem[key], val)
        self.seen[e][key] = val
        self.nins += 1

    def _deps(self, e, r, w):
        deps = {}
        for x in r:
            if x.lw is not None:
                k, v = x.lw
                if deps.get(k, 0) < v:
                    deps[k] = v
        for x in w:
            if x.lw is not None:
                k, v = x.lw
                if deps.get(k, 0) < v:
                    deps[k] = v
            for k, v in x.rd.items():
                if deps.get(k, 0) < v:
                    deps[k] = v
        for k, v in deps.items():
            if k == "pe" and e == "pe":
                continue
            self._wait(e, k, v)

    def _mark(self, key, val, r, w):
        for x in r:
            if x.rd.get(key, 0) < val:
                x.rd[key] = val
        for x in w:
            x.lw = (key, val)
            x.rd = {}

    def op(self, e, fn, r=(), w=()):
        self._deps(e, r, w)
        ins = fn(self.eng[e])
        self.cnt[e] += 1
        ins.then_inc(self.sem[e], 1)
        self._mark(e, self.cnt[e], r, w)
        self.nins += 1
        return ins

    def dma(self, q, out, in_, r=(), w=(), **kw):
        k = self.dpool[q][self.dnext[q] % len(self.dpool[q])]
        self.dnext[q] += 1
        self._wait(q, k, self.cnt[k])
        self._deps(q, r, w)
        ins = self.eng[q].dma_start(out=out, in_=in_, **kw)
        self.cnt[k] += 16
        ins.then_inc(self.sem[k], 16)
        self._mark(k, self.cnt[k], r, w)
        self.nins += 1
        return ins

    def barrier(self):
        for e in self.eng:
            for k, v in self.cnt.items():
                if k == e and e == "pe":
                    continue
                self._wait(e, k, v)

    def finish(self, e="sp"):
        for k, v in self.cnt.items():
            self._wait(e, k, v)


def gtile(r, hf):
    if hf == 0:
        return 2 * r if r % 2 == 0 else 2 * r + 1
    return 2 * r + 1 if r % 2 == 0 else 2 * r


def build(debug=False, stop_after=99):
    nc = bass.Bass("TRN2", target_bir_lowering=False)
    S = Sched(nc)

    def din(name, shape, dt=F32):
        return nc.dram_tensor(name, shape, dt, kind="ExternalInput").ap()

    def dscr(name, shape, dt):
        return nc.dram_tensor(name, shape, dt, kind="ExternalOutput" if debug else "Internal").ap()

    xall = din("xall", [S_ALL, D])
    xq = din("xq", [S_Q, D])
    cT = din("cT", [128, KC])
    w_ada = din("w_ada", [D, 6 * D])
    b_ada = din("b_ada", [1, 6 * D])
    n1g = din("n1g", [128, KC])
    n2g = din("n2g", [128, KC])
    w_in = din("w_in", [D, 12304])
    bfrep = din("bfrep", [128, 16])
    gains = din("gains", [128, 4])
    w_out = din("w_out", [D, D])
    w_pq = din("w_pq", [D, 2048])
    skT = din("skT", [128, 2048])
    uT = din("uT", [D, 16384])
    pv = din("pv", [16384, D])
    albias = din("albias", [128, 16 * 72])
    seld = din("sel", [128, 128])
    maskd = din("maskb", [128, 2048])
    pastd = din("pastb", [128, 64])
    End = din("En", [8, 1024])
    out = nc.dram_tensor("out", [S_Q, D], F32, kind="ExternalOutput").ap()

    mod_d = dscr("mod_d", [192, 128], F32)
    KT_d = dscr("KT_d", [32, 128, S_ALL], BF16)
    QT_d = dscr("QT_d", [32, 128, S_Q], BF16)
    V_d = dscr("V_d", [S_ALL, D], BF16)
    lf_d = dscr("lf_d", [S_ALL, 16], F32)
    x1_d = dscr("x1_d", [S_Q, D], F32)
    WgT_d = dscr("WgT_d", [16384, S_Q], BF16)
    R_scr = {k: Res(k) for k in ["mod_d", "KT_d", "QT_d", "V_d", "lf_d", "x1_d", "WgT_d", "out"]}

    w_ada_v = w_ada.rearrange("(kc p) n -> p kc n", p=128)
    w_in_v = w_in.rearrange("(kc p) n -> p kc n", p=128)
    w_out_v = w_out.rearrange("(kc p) n -> p kc n", p=128)
    w_pq_v = w_pq.rearrange("(kc p) n -> p kc n", p=128)
    uT_v = uT.rearrange("(kc p) n -> p kc n", p=128)

    def wload(dst, src_v, c0, cw, res, nsplit=4):
        step = KC // nsplit
        for i in range(nsplit):
            S.dma("pool", dst[:, i * step:(i + 1) * step, 0:cw], src_v[:, i * step:(i + 1) * step, c0:c0 + cw], w=[res])

    with ExitStack() as gs:
        def GT(name, shape, dt):
            return gs.enter_context(nc.sbuf_tensor(name, shape, dt))

        identf = GT("identf", [128, 128], F32)
        identb = GT("identb", [128, 128], BF16)
        onesb = GT("onesb", [128, 128], BF16)
        modT = GT("modT", [128, 192], F32)
        A1 = GT("A1", [128, KC], F32)
        A2 = GT("A2", [128, KC], F32)
        gn = GT("gn", [128, 2 * KC], F32)
        gainsb = GT("gainsb", [128, 4], F32)
        cb = GT("cb", [128, KC], BF16)
        r_c = Res("c")
        r_const = Res("const")
        r_mod = Res("modT")

        def ada_group(ng, wa_b, brow_b, orow_b, psrow, r_wa_b, r_brow_b, r_orow_b, r_ps_b):
            S.dma("sp", brow_b[:], b_ada[0:1, ng * 512:(ng + 1) * 512], w=[r_brow_b])
            for kc in range(KC):
                S.op("pe", lambda e, kc=kc: e.matmul(psrow, lhsT=cb[:, kc:kc + 1], rhs=wa_b[:, kc, :],
                                                     start=(kc == 0), stop=(kc == KC - 1)), r=[r_c, r_wa_b], w=[r_ps_b])
            S.op("dve", lambda e: e.tensor_tensor(out=orow_b[:], in0=psrow, in1=brow_b[:], op=ALU.add),
                 r=[r_ps_b, r_brow_b], w=[r_orow_b])
            S.dma("sp", mod_d[ng * 4:(ng + 1) * 4, :].rearrange("(o j) p -> o (j p)", o=1), orow_b[:],
                  r=[r_orow_b], w=[R_scr["mod_d"]])

        def build_modT(lo, hi, tag):
            with ExitStack() as esm:
                mrow = esm.enter_context(nc.sbuf_tensor("mrow" + tag, [64, 256], F32))
                psm = esm.enter_context(nc.psum_tensor("psm" + tag, [128, 128], F32))
                r_m = Res()
                n = (hi - lo) // 64
                for i in range(n):
                    S.dma("sp", mrow[:, i * 128:(i + 1) * 128], mod_d[lo + i * 64:lo + (i + 1) * 64, :], r=[R_scr["mod_d"]], w=[r_m])
                for i in range(n):
                    S.op("pe", lambda e, i=i: e.matmul(psm[:, i * 64:(i + 1) * 64], lhsT=mrow[:, i * 128:(i + 1) * 128],
                                                       rhs=identf[0:64, 0:64], start=True, stop=True), r=[r_m, r_const], w=[r_mod])
                S.op("dve", lambda e: e.tensor_copy(out=modT[:, lo:hi], in_=psm[:, 0:hi - lo]), r=[r_mod], w=[r_mod])
                S.barrier()

        S.op("pool", lambda e: e.memset(identf[:], 0.0), w=[r_const])
        S.op("pool", lambda e: e.affine_select(out=identf[:], in_=identf[:], pattern=[[-1, 128]], compare_op=ALU.not_equal,
                                               fill=1.0, base=0, channel_multiplier=1), r=[r_const], w=[r_const])
        S.op("dve", lambda e: e.tensor_copy(out=identb[:], in_=identf[:]), r=[r_const], w=[r_const])
        S.op("dve", lambda e: e.memset(onesb[:], 1.0), w=[r_const])
        S.dma("sp", gn[:, 0:KC], n1g[:, :], w=[r_const])
        S.dma("sp", gn[:, KC:2 * KC], n2g[:, :], w=[r_const])
        S.dma("sp", gainsb[:], gains[:, :], w=[r_const])

        with ExitStack() as es:
            def T(name, shape, dt):
                return es.enter_context(nc.sbuf_tensor(name, shape, dt))

            def P(name, shape, dt):
                return es.enter_context(nc.psum_tensor(name, shape, dt))

            cf = T("cf", [128, KC], F32)
            wa = [T("wa%d" % i, [128, KC, 512], BF16) for i in range(2)]
            brow = [T("brow%d" % i, [1, 512], F32) for i in range(2)]
            orow = [T("orow%d" % i, [1, 512], F32) for i in range(2)]
            psr = [P("psr%d" % i, [1, 512], F32) for i in range(2)]
            r_wa = [Res(), Res()]
            r_brow = [Res(), Res()]
            r_orow = [Res(), Res()]
            r_psr = [Res(), Res()]
            S.dma("sp", cf[:], cT[:, :], w=[r_c])
            S.op("act", lambda e: e.activation(out=cb[:], in_=cf[:], func=AF.Silu), r=[r_c], w=[r_c])
            NG0 = 16
            wload(wa[0], w_ada_v, 0, 512, r_wa[0])
            for ng in range(NG0):
                b = ng % 2
                if ng + 1 < NG0:
                    wload(wa[1 - b], w_ada_v, (ng + 1) * 512, 512, r_wa[1 - b])
                ada_group(ng, wa[b], brow[b], orow[b], psr[b][0:1, :], r_wa[b], r_brow[b], r_orow[b], r_psr[b])
            S.barrier()
        build_modT(0, 64, "a")
        S.op("dve", lambda e: e.scalar_tensor_tensor(out=A1[:], in0=modT[:, KC:2 * KC], scalar=1.0, in1=gn[:, 0:KC],
                                                     op0=ALU.add, op1=ALU.mult), r=[r_mod, r_const], w=[r_mod])
        S.barrier()
        if stop_after <= 0:
            S.finish()
            return nc

        def build_hT(es, hT, r_hT, src, row0, Acol, shcol, tagname):
            def T(name, shape, dt):
                return es.enter_context(nc.sbuf_tensor(tagname + name, shape, dt))

            def P(name, shape, dt):
                return es.enter_context(nc.psum_tensor(tagname + name, shape, dt))

            xt = [T("xt%d" % i, [128, D], F32) for i in range(2)]
            xn = [T("xn%d" % i, [128, D], BF16) for i in range(2)]
            junk = T("junk", [128, D], BF16)
            st = T("st", [128, 16], F32)
            pst = [P("pst%d" % i, [128, 1024], BF16) for i in range(2)]
            r_xt = [Res(), Res()]
            r_xn = [Res(), Res()]
            r_junk = Res()
            r_st = Res()
            r_pst = [Res(), Res()]
            S.dma("sp", xt[0][:], src[row0:row0 + 128, :], w=[r_xt[0]])
            k = 0
            for tt in range(8):
                b = tt % 2
                if tt + 1 < 8:
                    S.dma("sp", xt[1 - b][:], src[row0 + (tt + 1) * 128:row0 + (tt + 2) * 128, :], w=[r_xt[1 - b]])
                S.op("dve", lambda e, b=b: e.memset(st[:, 2 * b:2 * b + 1], 0.0), w=[r_st])
                S.op("act", lambda e, b=b, tt=tt: e.activation(out=junk[:], in_=xt[b][:], func=AF.Square, accum_out=st[:, 2 * b:2 * b + 1]),
                     r=[r_xt[b]], w=[r_junk, r_st])
                S.op("act", lambda e, b=b: e.activation(out=st[:, 2 * b + 1:2 * b + 2], in_=st[:, 2 * b:2 * b + 1], func=AF.Sqrt,
                                                        bias=EPS, scale=1.0 / D), r=[r_st], w=[r_st])
                S.op("dve", lambda e, b=b: e.reciprocal(out=st[:, 2 * b + 1:2 * b + 2], in_=st[:, 2 * b + 1:2 * b + 2]), r=[r_st], w=[r_st])
                S.op("dve", lambda e, b=b: e.tensor_scalar(out=xn[b][:], in0=xt[b][:], scalar1=st[:, 2 * b + 1:2 * b + 2], scalar2=None,
                                                           op0=ALU.mult), r=[r_xt[b], r_st], w=[r_xn[b]])
                for q4 in range(4):
                    pb = k % 2
                    k += 1
                    for i in range(8):
                        kc = q4 * 8 + i
                        S.op("pe", lambda e, pb=pb, i=i, kc=kc, b=b: e.transpose(out=pst[pb][:, i * 128:(i + 1) * 128],
                                                                                 in_=xn[b][:, kc * 128:(kc + 1) * 128], identity=identb[:]),
                             r=[r_xn[b], r_const], w=[r_pst[pb]])
                    for i in range(8):
                        kc = q4 * 8 + i
                        if False:
                            S.op("act", lambda e, pb=pb, i=i, kc=kc, tt=tt: e.activation(
                                out=hT[:, kc, tt * 128:(tt + 1) * 128], in_=pst[pb][:, i * 128:(i + 1) * 128], func=AF.Identity,
                                bias=shcol[:, kc:kc + 1], scale=Acol[:, kc:kc + 1]),
                                 r=[r_pst[pb], r_mod], w=[r_hT[tt][kc]])
                        else:
                            S.op("dve", lambda e, pb=pb, i=i, kc=kc, tt=tt: e.tensor_scalar(
                                out=hT[:, kc, tt * 128:(tt + 1) * 128], in0=pst[pb][:, i * 128:(i + 1) * 128],
                                scalar1=Acol[:, kc:kc + 1], scalar2=shcol[:, kc:kc + 1], op0=ALU.mult, op1=ALU.add),
                                 r=[r_pst[pb], r_mod], w=[r_hT[tt][kc]])
            S.barrier()

        passes = [(xall, 0, 0, "kv"), (xall, 1024, 1024, "kv"), (xq, 0, 0, "q")]
        for pi, (src, row0, tok0, kind) in enumerate(passes):
            with ExitStack() as es:
                def T(name, shape, dt):
                    return es.enter_context(nc.sbuf_tensor("p%d_%s" % (pi, name), shape, dt))

                def P(name, shape, dt):
                    return es.enter_context(nc.psum_tensor("p%d_%s" % (pi, name), shape, dt))

                hT = T("hT", [128, KC, 1024], BF16)
                r_hT = [[Res() for _ in range(KC)] for _ in range(8)] if os.environ.get('FINE_HT', '1') == '1' else [[Res()] * KC for _ in range(8)]
                wg = [T("wg%d" % i, [128, KC, 512], BF16) for i in range(2)]
                r_wg = [Res(), Res()]
                first_c0 = 2048 if kind == "kv" else 0
                wload(wg[0], w_in_v, first_c0, 512, r_wg[0])
                with ExitStack() as es2:
                    build_hT(es2, hT, r_hT, src, row0, A1, modT[:, 0:KC], "p%d_" % pi)
                psk = [P("psk%d" % i, [128, 512], F32) for i in range(2)]
                pss = [P("pss%d" % i, [128, 512], F32) for i in range(2)]
                r_psk = [Res(), Res()]
                r_pss = [Res(), Res()]
                sq = [T("sq%d" % i, [128, 512], BF16) for i in range(2)]
                sd = [T("sd%d" % i, [128, 512], F32) for i in range(2)]
                kn = [T("kn%d" % i, [128, 512], BF16) for i in range(2)]
                r_sq = [Res(), Res()]
                r_sd = [Res(), Res()]
                r_kn = [Res(), Res()]
                vb = [T("vb%d" % i, [128, 512], BF16) for i in range(2)]
                r_vb = [Res(), Res()]
                groups = []
                if kind == "kv":
                    for g in range(4):
                        groups.append((2048 + g * 512, "k", g * 4, 1))
                    for g in range(4):
                        groups.append((8192 + g * 512, "k", 16 + g * 4, 3))
                    for g in range(4):
                        groups.append((4096 + g * 512, "v", g * 4, 0))
                    for g in range(4):
                        groups.append((10240 + g * 512, "v", 16 + g * 4, 0))
                else:
                    for g in range(4):
                        groups.append((g * 512, "q", g * 4, 0))
                    for g in range(4):
                        groups.append((6144 + g * 512, "q", 16 + g * 4, 2))
                assert groups[0][0] == first_c0
                u = 0
                vcount = 0
                for gi, (c0, typ, head0, gcol) in enumerate(groups):
                    b = gi % 2
                    if gi + 1 < len(groups):
                        wload(wg[1 - b], w_in_v, groups[gi + 1][0], 512, r_wg[1 - b])
                    if typ in ("k", "q"):
                        dst = KT_d if typ == "k" else QT_d
                        rdst = R_scr["KT_d"] if typ == "k" else R_scr["QT_d"]
                        for hh in range(4):
                            for tg in range(2):
                                pb = u % 2
                                u += 1
                                for kc in range(KC):
                                    S.op("pe", lambda e, pb=pb, kc=kc, hh=hh, tg=tg, b=b: e.matmul(
                                        psk[pb][:], lhsT=wg[b][:, kc, hh * 128:(hh + 1) * 128], rhs=hT[:, kc, tg * 512:(tg + 1) * 512],
                                        start=(kc == 0), stop=(kc == KC - 1)),
                                         r=[r_wg[b]] + [r_hT[t_][kc] for t_ in range(tg * 4, tg * 4 + 4)], w=[r_psk[pb]])
                                S.op("act", lambda e, pb=pb: e.activation(out=sq[pb][:], in_=psk[pb][:], func=AF.Square),
                                     r=[r_psk[pb]], w=[r_sq[pb]])
                                S.op("pe", lambda e, pb=pb: e.matmul(pss[pb][:], lhsT=onesb[:], rhs=sq[pb][:], start=True, stop=True),
                                     r=[r_sq[pb], r_const], w=[r_pss[pb]])
                                S.op("act", lambda e, pb=pb: e.activation(out=sd[pb][:], in_=pss[pb][:], func=AF.Sqrt, bias=EPS, scale=1.0 / 128),
                                     r=[r_pss[pb]], w=[r_sd[pb]])
                                S.op("dve", lambda e, pb=pb: e.reciprocal(out=sd[pb][:], in_=sd[pb][:]), r=[r_sd[pb]], w=[r_sd[pb]])
                                S.op("dve", lambda e, pb=pb, gcol=gcol: e.scalar_tensor_tensor(
                                    out=kn[pb][:], in0=psk[pb][:], scalar=gainsb[:, gcol:gcol + 1], in1=sd[pb][:], op0=ALU.mult, op1=ALU.mult),
                                     r=[r_psk[pb], r_sd[pb], r_const], w=[r_kn[pb]])
                                t0 = tok0 + tg * 512
                                S.dma("sp", dst[head0 + hh, :, t0:t0 + 512], kn[pb][:], r=[r_kn[pb]], w=[rdst])
                    else:
                        for tt in range(8):
                            pb = u % 2
                            u += 1
                            for kc in range(KC):
                                S.op("pe", lambda e, pb=pb, kc=kc, tt=tt, b=b: e.matmul(
                                    psk[pb][:], lhsT=hT[:, kc, tt * 128:(tt + 1) * 128], rhs=wg[b][:, kc, :],
                                    start=(kc == 0), stop=(kc == KC - 1)), r=[r_wg[b], r_hT[tt][kc]], w=[r_psk[pb]])
                            vbi = vcount % 2
                            vcount += 1
                            S.op("act", lambda e, pb=pb, vbi=vbi: e.activation(out=vb[vbi][:], in_=psk[pb][:], func=AF.Copy),
                                 r=[r_psk[pb]], w=[r_vb[vbi]])
                            S.dma("sp", V_d[tok0 + tt * 128:tok0 + (tt + 1) * 128, head0 * 128:head0 * 128 + 512], vb[vbi][:],
                                  r=[r_vb[vbi]], w=[R_scr["V_d"]])
                if kind == "kv":
                    wf = T("wf", [128, KC, 16], BF16)
                    bfs = T("bfs", [128, 16], F32)
                    zt = [T("zt%d" % i, [128, 16], F32) for i in range(2)]
                    r_wf = Res()
                    r_zt = [Res(), Res()]
                    S.dma("pool", wf[:], w_in_v[:, :, 12288:12304], w=[r_wf])
                    S.dma("sp", bfs[:], bfrep[:, :], w=[r_wf])
                    for tt in range(8):
                        pb = u % 2
                        u += 1
                        zb = tt % 2
                        for kc in range(KC):
                            S.op("pe", lambda e, pb=pb, kc=kc, tt=tt: e.matmul(
                                psk[pb][:, 0:16], lhsT=hT[:, kc, tt * 128:(tt + 1) * 128], rhs=wf[:, kc, :],
                                start=(kc == 0), stop=(kc == KC - 1)), r=[r_wf, r_hT[tt][kc]], w=[r_psk[pb]])
                        S.op("dve", lambda e, pb=pb, zb=zb: e.tensor_tensor(out=zt[zb][:], in0=psk[pb][:, 0:16], in1=bfs[:], op=ALU.add),
                             r=[r_psk[pb], r_wf], w=[r_zt[zb]])
                        S.op("act", lambda e, zb=zb: e.activation(out=zt[zb][:], in_=zt[zb][:], func=AF.Exp, scale=-1.0), r=[r_zt[zb]], w=[r_zt[zb]])
                        S.op("act", lambda e, zb=zb: e.activation(out=zt[zb][:], in_=zt[zb][:], func=AF.Ln, bias=1.0), r=[r_zt[zb]], w=[r_zt[zb]])
                        S.dma("sp", lf_d[tok0 + tt * 128:tok0 + (tt + 1) * 128, :], zt[zb][:], r=[r_zt[zb]], w=[R_scr["lf_d"]])
                S.barrier()
        if stop_after <= 1:
            S.finish()
            return nc

        mixT_cm = nc.sbuf_tensor("mixT", [128, KC, S_Q], BF16)
        mixT = mixT_cm.__enter__()
        r_mix = [Res() for _ in range(8)]
        with ExitStack() as es:
            def T(name, shape, dt):
                return es.enter_context(nc.sbuf_tensor("a_" + name, shape, dt))

            def P(name, shape, dt):
                return es.enter_context(nc.psum_tensor("a_" + name, shape, dt))

            biasT = T("biasT", [128, 32, 72], F32)
            maskf = T("maskf", [128, 2048], F32)
            maskb = T("maskb", [128, 2048], BF16)
            pastb = T("pastb", [128, 64], F32)
            Enf = T("Enf", [8, 1024], F32)
            Enb = T("Enb", [8, 1024], BF16)
            selt = T("selt", [128, 128], F32)
            lt = T("lt", [128, 16, 16], F32)
            Lkey = T("Lkey", [128, 16, 16], F32)
            Lmid = T("Lmid", [128, 16, 16], F32)
            Lref = T("Lref", [128, 8, 16], F32)
            ltmp = T("ltmp", [128, 16, 16], F32)
            triu = T("triu", [128, 128], F32)
            onesf = T("onesf", [128, 128], F32)
            e64 = T("e64", [128, 128], F32)
            r_s = Res("attn_setup")
            psL = P("psL", [128, 512], F32)
            r_psL = Res()
            S.dma("sp", biasT[:, 0:16, :].rearrange("p h c -> p (h c)"), albias[:, :], w=[r_s])
            S.dma("sp", maskf[:], maskd[:, :], w=[r_s])
            S.dma("sp", pastb[:], pastd[:, :], w=[r_s])
            S.dma("sp", Enf[:], End[:, :], w=[r_s])
            S.dma("sp", selt[:], seld[:, :], w=[r_s])
            S.dma("sp", lt[:], lf_d.rearrange("(j p) h -> p j h", p=128), r=[R_scr["lf_d"]], w=[r_s])
            S.op("dve", lambda e: e.tensor_copy(out=maskb[:], in_=maskf[:]), r=[r_s], w=[r_s])
            S.op("dve", lambda e: e.tensor_copy(out=Enb[:], in_=Enf[:]), r=[r_s], w=[r_s])
            S.op("pool", lambda e: e.memset(onesf[:], 1.0), w=[r_s])
            S.op("pool", lambda e: e.memset(triu[:], 1.0), w=[r_s])
            S.op("pool", lambda e: e.affine_select(out=triu[:], in_=triu[:], pattern=[[1, 128]], compare_op=ALU.is_ge,
                                                   fill=0.0, base=0, channel_multiplier=-1), r=[r_s], w=[r_s])
            S.op("pool", lambda e: e.memset(e64[:], 0.0), w=[r_s])
            S.op("pool", lambda e: e.affine_select(out=e64[:], in_=e64[:], pattern=[[0, 128]], compare_op=ALU.not_equal,
                                                   fill=1.0, base=-64, channel_multiplier=1), r=[r_s], w=[r_s])
            for j in range(16):
                for i in range(j + 1):
                    S.op("pe", lambda e, i=i, j=j: e.matmul(psL[:, 0:16], lhsT=(triu[:] if i == j else onesf[:]), rhs=lt[:, i, :],
                                                            start=(i == 0), stop=(i == j)), r=[r_s], w=[r_psL])
                S.op("dve", lambda e, j=j: e.tensor_copy(out=Lkey[:, j, :], in_=psL[:, 0:16]), r=[r_psL], w=[r_s])
            S.op("pe", lambda e: e.matmul(psL[:, 0:256], lhsT=e64[:], rhs=Lkey[:].rearrange("p j h -> p (j h)"), start=True, stop=True),
                 r=[r_s], w=[r_psL])
            S.op("dve", lambda e: e.tensor_copy(out=Lmid[:].rearrange("p j h -> p (j h)"), in_=psL[:, 0:256]), r=[r_psL], w=[r_s])
            for r in range(8):
                S.op("dve", lambda e, r=r: e.tensor_tensor(
                    out=ltmp[:], in0=Lmid[:], in1=selt[:, r * 16:(r + 1) * 16].rearrange("p (g o) -> p g o", o=1).to_broadcast([128, 16, 16]),
                    op=ALU.mult), r=[r_s], w=[r_s])
                S.op("dve", lambda e, r=r: e.tensor_reduce(out=Lref[:, r, :], in_=ltmp[:].rearrange("p g h -> p h g"), axis=AX.X, op=ALU.add),
                     r=[r_s], w=[r_s])
            for h in range(16):
                for r in range(8):
                    nk = 2 * r + 2
                    S.op("dve", lambda e, h=h, r=r, nk=nk: e.tensor_scalar(
                        out=biasT[:, 16 + h, OFF[r]:OFF[r] + nk], in0=Lkey[:, 0:nk, h], scalar1=Lref[:, r, h:h + 1], scalar2=None,
                        op0=ALU.subtract), r=[r_s], w=[r_s])

            wa2 = [T("wa2_%d" % i, [128, KC, 512], BF16) for i in range(2)]
            brow2 = [T("brow2_%d" % i, [1, 512], F32) for i in range(2)]
            orow2 = [T("orow2_%d" % i, [1, 512], F32) for i in range(2)]
            r_wa2 = [Res(), Res()]
            r_brow2 = [Res(), Res()]
            r_orow2 = [Res(), Res()]
            wload(wa2[0], w_ada_v, 16 * 512, 512, r_wa2[0])
            KT = [T("KT%d" % i, [128, S_ALL], BF16) for i in range(2)]
            QT = [T("QT%d" % i, [128, S_Q], BF16) for i in range(2)]
            Vh = [T("Vh%d" % i, [128, 16, 128], BF16) for i in range(2)]
            r_KT = [Res(), Res()]
            r_QT = [Res(), Res()]
            r_Vh = [Res(), Res()]
            PT = [T("PT%d" % i, [128, 1024], BF16) for i in range(2)]
            r_PT = [[Res() for _ in range(8)] for _ in range(2)] if os.environ.get('FINE_PT', '1') == '1' else [[Res()] * 8 for _ in range(2)]
            psS = [P("psS%d" % i, [128, 1024], F32) for i in range(2)]
            r_psS = [[Res() for _ in range(8)] for _ in range(2)] if os.environ.get('FINE_ATT', '0') == '1' else [[Res()] * 8 for _ in range(2)]
            psOZ = [P("psOZ%d" % i, [128, 256], F32) for i in range(2)]
            r_psOZ = [Res(), Res()]
            psG = P("psG", [128, 128], F32)
            r_psG = Res()
            kmf = T("kmf", [128, 8], F32)
            kmb = T("kmb", [128, 8], BF16)
            gsb = T("gsb", [128, 8], F32)
            g8 = T("g8", [128, 8], F32)
            selb = T("selb", [128, 8], F32)
            selT = [T("selT%d" % i, [8, 128], BF16) for i in range(2)]
            r_g = Res()
            r_selT = [Res(), Res()]
            rec = T("rec", [128, 128], F32)
            r_rec = Res()
            V_v = V_d.rearrange("(j p) c -> p j c", p=128)

            def load_head(H, b):
                S.dma("sp", KT[b][:], KT_d[H], r=[R_scr["KT_d"]], w=[r_KT[b]])
                S.dma("sp", QT[b][:], QT_d[H], r=[R_scr["QT_d"]], w=[r_QT[b]])
                S.dma("sp", Vh[b][:], V_v[:, :, H * 128:(H + 1) * 128], r=[R_scr["V_d"]], w=[r_Vh[b]])

            scale = 128.0 ** -0.5
            units = []
            for H in range(32):
                for r in range(8):
                    nk = 2 * r + 2
                    for c0 in range(0, nk, 8):
                        units.append((H, r, c0, list(range(c0, min(c0 + 8, nk)))))
            gstate = {"gcount": 0, "sb": {}}

            def emit_scores(ui):
                H, r, c0, tiles = units[ui]
                b = H % 2
                ub = ui % 2
                nk = 2 * r + 2
                moba = H < 16
                gate = moba and r >= 4
                if r == 0 and c0 == 0:
                    g_ = 16 + H
                    gb = g_ % 2
                    if g_ + 1 < 48:
                        wload(wa2[1 - gb], w_ada_v, (g_ + 1) * 512, 512, r_wa2[1 - gb])
                    ada_group(g_, wa2[gb], brow2[gb], orow2[gb], psL[0:1, :], r_wa2[gb], r_brow2[gb], r_orow2[gb], r_psL)
                    if moba:
                        S.op("dve", lambda e: e.tensor_reduce(out=kmf[:], in_=KT[b][:].rearrange("d (n s) -> d n s", s=256), axis=AX.X, op=ALU.add),
                             r=[r_KT[b]], w=[r_g])
                        S.op("dve", lambda e: e.tensor_scalar(out=kmb[:], in0=kmf[:], scalar1=1.0 / 256, scalar2=None, op0=ALU.mult), r=[r_g], w=[r_g])
                if gate and c0 == 0:
                    sb = gstate["gcount"] % 2
                    gstate["gcount"] += 1
                    gstate["sb"][(H, r)] = sb
                    S.op("pe", lambda e: e.matmul(psG[:, 0:8], lhsT=QT[b][:, r * 128:(r + 1) * 128], rhs=kmb[:, 0:8], start=True, stop=True),
                         r=[r_QT[b], r_g], w=[r_psG])
                    S.op("dve", lambda e: e.tensor_tensor(out=gsb[:], in0=psG[:, 0:8], in1=pastb[:, r * 8:(r + 1) * 8], op=ALU.add),
                         r=[r_psG, r_s], w=[r_g])
                    S.op("dve", lambda e: e.max(out=g8[:], in_=gsb[:]), r=[r_g], w=[r_g])
                    S.op("dve", lambda e: e.tensor_scalar(out=selb[:], in0=gsb[:], scalar1=g8[:, 2:3], scalar2=NEG, op0=ALU.is_lt, op1=ALU.mult),
                         r=[r_g], w=[r_g])
                    S.op("pe", lambda e: e.matmul(psG[0:8, 0:128], lhsT=selb[:], rhs=identf[:], start=True, stop=True),
                         r=[r_g, r_const], w=[r_psG])
                    S.op("act", lambda e: e.activation(out=selT[sb][:], in_=psG[0:8, 0:128], func=AF.Copy), r=[r_psG], w=[r_selT[sb]])
                sb = gstate["sb"].get((H, r), 0)
                for j in tiles:
                    o = (j - c0) * 128
                    is_mask = j >= nk - 2
                    is_sel = gate and not is_mask
                    extra = is_mask or is_sel
                    S.op("pe", lambda e: e.matmul(
                        psS[ub][:, o:o + 128], lhsT=KT[b][:, j * 128:(j + 1) * 128], rhs=QT[b][:, r * 128:(r + 1) * 128],
                        start=True, stop=(not extra)), r=[r_KT[b], r_QT[b]], w=[r_psS[ub][j - c0]])
                    if is_mask:
                        mi = r * 2 + (j - (nk - 2))
                        S.op("pe", lambda e: e.matmul(
                            psS[ub][:, o:o + 128], lhsT=identb[:], rhs=maskb[:, mi * 128:(mi + 1) * 128], start=False, stop=True),
                             r=[r_s, r_const], w=[r_psS[ub][j - c0]])
                    elif is_sel:
                        n = j // 2
                        S.op("pe", lambda e: e.matmul(
                            psS[ub][:, o:o + 128], lhsT=Enb[:, n * 128:(n + 1) * 128], rhs=selT[sb][:], start=False, stop=True),
                             r=[r_s, r_selT[sb]], w=[r_psS[ub][j - c0]])

            def emit_exps(ui):
                H, r, c0, tiles = units[ui]
                ub = ui % 2
                for j in tiles:
                    o = (j - c0) * 128
                    S.op("act", lambda e: e.activation(
                        out=PT[ub][:, o:o + 128], in_=psS[ub][:, o:o + 128], func=AF.Exp,
                        bias=biasT[:, H, OFF[r] + j:OFF[r] + j + 1], scale=scale), r=[r_psS[ub][j - c0], r_s], w=[r_PT[ub][j - c0]])

            def emit_pv(ui):
                H, r, c0, tiles = units[ui]
                b = H % 2
                ub = ui % 2
                nk = 2 * r + 2
                ob = (H * 8 + r) % 2
                for j in tiles:
                    o = (j - c0) * 128
                    S.op("pe", lambda e: e.matmul(
                        psOZ[ob][:, 0:128], lhsT=Vh[b][:, j, :], rhs=PT[ub][:, o:o + 128], start=(j == 0), stop=(j == nk - 1)),
                         r=[r_Vh[b], r_PT[ub][j - c0]], w=[r_psOZ[ob]])
                    S.op("pe", lambda e: e.matmul(
                        psOZ[ob][:, 128:256], lhsT=onesb[:], rhs=PT[ub][:, o:o + 128], start=False, stop=(j == nk - 1), skip_group_check=True),
                         r=[r_const, r_PT[ub][j - c0]], w=[r_psOZ[ob]])
                if tiles[-1] == nk - 1:
                    S.op("dve", lambda e: e.reciprocal(out=rec[:], in_=psOZ[ob][:, 128:256]), r=[r_psOZ[ob]], w=[r_rec])
                    S.op("dve", lambda e: e.tensor_tensor(out=mixT[:, H, r * 128:(r + 1) * 128], in0=psOZ[ob][:, 0:128], in1=rec[:], op=ALU.mult),
                         r=[r_psOZ[ob], r_rec], w=[r_mix[r]])
                    if r == 7 and H + 2 < 32:
                        load_head(H + 2, b)

            load_head(0, 0)
            load_head(1, 1)
            emit_scores(0)
            for ui in range(len(units)):
                if ui + 1 < len(units):
                    emit_scores(ui + 1)
                emit_exps(ui)
                emit_pv(ui)
            S.barrier()
        build_modT(64, 192, "b")
        S.op("dve", lambda e: e.scalar_tensor_tensor(out=A2[:], in0=modT[:, 4 * KC:5 * KC], scalar=1.0, in1=gn[:, KC:2 * KC],
                                                     op0=ALU.add, op1=ALU.mult), r=[r_mod, r_const], w=[r_mod])
        S.barrier()
        if stop_after <= 2:
            S.finish()
            mixT_cm.__exit__(None, None, None)
            return nc

        with ExitStack() as es:
            def T(name, shape, dt):
                return es.enter_context(nc.sbuf_tensor("o_" + name, shape, dt))

            def P(name, shape, dt):
                return es.enter_context(nc.psum_tensor("o_" + name, shape, dt))

            g1rep = T("g1rep", [128, D], F32)
            r_g1 = Res()
            for i in range(4):
                S.dma("sp", g1rep[:, i * 1024:(i + 1) * 1024],
                      mod_d[64 + i * 8:64 + (i + 1) * 8, :].rearrange("(o j) p -> o (j p)", o=1).partition_broadcast(128),
                      r=[R_scr["mod_d"]], w=[r_g1])
            wo = [T("wo%d" % i, [128, KC, 512], BF16) for i in range(2)]
            r_wo = [Res(), Res()]
            pso = [P("pso%d" % i, [128, 512], F32) for i in range(2)]
            r_pso = [Res(), Res()]
            xs = [T("xs%d" % i, [128, 512], F32) for i in range(2)]
            t1 = [T("t1%d" % i, [128, 512], F32) for i in range(2)]
            r_xs = [Res(), Res()]
            r_t1 = [Res(), Res()]
            wload(wo[0], w_out_v, 0, 512, r_wo[0])
            u = 0
            for ng in range(8):
                b = ng % 2
                if ng + 1 < 8:
                    wload(wo[1 - b], w_out_v, (ng + 1) * 512, 512, r_wo[1 - b])
                for tt in range(8):
                    pb = u % 2
                    u += 1
                    S.dma("sp", xs[pb][:], xq[tt * 128:(tt + 1) * 128, ng * 512:(ng + 1) * 512], w=[r_xs[pb]])
                    for kc in range(KC):
                        S.op("pe", lambda e, pb=pb, kc=kc, tt=tt, b=b: e.matmul(
                            pso[pb][:], lhsT=mixT[:, kc, tt * 128:(tt + 1) * 128], rhs=wo[b][:, kc, :], start=(kc == 0), stop=(kc == KC - 1)),
                             r=[r_wo[b], r_mix[tt]], w=[r_pso[pb]])
                    S.op("dve", lambda e, pb=pb, ng=ng: e.tensor_tensor(out=t1[pb][:], in0=pso[pb][:], in1=g1rep[:, ng * 512:(ng + 1) * 512], op=ALU.mult),
                         r=[r_pso[pb], r_g1], w=[r_t1[pb]])
                    S.op("dve", lambda e, pb=pb: e.tensor_tensor(out=t1[pb][:], in0=t1[pb][:], in1=xs[pb][:], op=ALU.add),
                         r=[r_t1[pb], r_xs[pb]], w=[r_t1[pb]])
                    S.dma("sp", x1_d[tt * 128:(tt + 1) * 128, ng * 512:(ng + 1) * 512], t1[pb][:], r=[r_t1[pb]], w=[R_scr["x1_d"]])
            S.barrier()
        mixT_cm.__exit__(None, None, None)
        if stop_after <= 3:
            S.finish()
            return nc

        with ExitStack() as es:
            def T(name, shape, dt):
                return es.enter_context(nc.sbuf_tensor("e_" + name, shape, dt))

            def P(name, shape, dt):
                return es.enter_context(nc.psum_tensor("e_" + name, shape, dt))

            h2T = T("h2T", [128, KC, S_Q], BF16)
            r_h2 = [[Res() for _ in range(KC)] for _ in range(8)]
            with ExitStack() as es2:
                build_hT(es2, h2T, r_h2, x1_d, 0, A2, modT[:, 3 * KC:4 * KC], "e_")
            RC = T("RC", [128, 8, 8, 2, 128], F32)
            tau2 = T("tau2", [128, 64], F32)
            r_RC = [[Res() for _ in range(8)] for _ in range(8)]
            with ExitStack() as es2:
                def T2(name, shape, dt):
                    return es2.enter_context(nc.sbuf_tensor("e7_" + name, shape, dt))

                def P2(name, shape, dt):
                    return es2.enter_context(nc.psum_tensor("e7_" + name, shape, dt))

                skf = T2("skf", [128, 2048], F32)
                skb = T2("skb", [128, 2048], BF16)
                r_sk = Res()
                S.dma("sp", skf[:], skT[:, :], w=[r_sk])
                S.op("dve", lambda e: e.tensor_copy(out=skb[:], in_=skf[:]), r=[r_sk], w=[r_sk])
                wq = [T2("wq%d" % i, [128, KC, 256], BF16) for i in range(2)]
                r_wq = [Res(), Res()]
                psq = [P2("psq%d" % i, [128, 512], F32) for i in range(2)]
                r_psq = [Res(), Res()]
                qTb = [T2("qTb%d" % i, [128, 1024], BF16) for i in range(4)]
                r_qT = [Res() for _ in range(4)]
                NCH = 3
                pssc = [[P2("pssc%d_%d" % (i, c), [128, 128], F32) for c in range(2)] for i in range(NCH)]
                sets = []
                for i in range(NCH):
                    sets.append(dict(
                        scs=[T2("scs%d_%d" % (i, c), [128, 128], F32) for c in range(2)],
                        m8=T2("m8_%d" % i, [128, 8, 8], F32), rv=T2("rv%d" % i, [128, 2, 16], F32),
                        tmpR=T2("tmpR%d" % i, [128, 128], F32), G=T2("G%d" % i, [128, 256], F32), G2=T2("G2_%d" % i, [128, 256], F32),
                        zz=T2("zz%d" % i, [128, 8], F32), pssc=pssc[i],
                        r_pssc=[Res(), Res()], r_scs=[Res(), Res()], r_m=[Res(), Res()], r_rv=[Res(), Res()], r_t=[Res()] * 2, r_g=Res()))

                def chain(tt, h, st):
                    m8, rv, tmpR, G, G2, zz = st["m8"], st["rv"], st["tmpR"], st["G"], st["G2"], st["zz"]
                    rr = r_RC[tt][h]
                    for c in range(2):
                        hc = h * 2 + c
                        scs = st["scs"][c]
                        Rc = RC[:, tt, h, c, :]
                        r_ps, r_sc, r_m, r_rv, r_t = st["r_pssc"][c], st["r_scs"][c], st["r_m"][c], st["r_rv"][c], st["r_t"][c]
                        qi = (h % 2) * 2 + c
                        S.op("pe", lambda e: e.matmul(st["pssc"][c][:], lhsT=qTb[qi][:, tt * 128:(tt + 1) * 128], rhs=skb[:, hc * 128:(hc + 1) * 128],
                                                      start=True, stop=True), r=[r_qT[qi], r_sk], w=[r_ps])
                        yield
                        S.op("act", lambda e: e.activation(out=scs[:], in_=st["pssc"][c][:], func=AF.Copy), r=[r_ps], w=[r_sc])
                        yield
                        S.op("dve", lambda e: e.max(out=m8[:, c, :], in_=scs[:]), r=[r_sc], w=[r_m])
                        yield
                        S.op("dve", lambda e: e.tensor_scalar(out=m8[:, 4 + c, 0:1], in0=m8[:, c, 0:1], scalar1=-1.0, scalar2=None, op0=ALU.mult),
                             r=[r_m], w=[r_m])
                        yield
                        S.op("act", lambda e: e.activation(out=Rc, in_=scs[:], func=AF.Exp, bias=m8[:, 4 + c, 0:1], scale=1.0), r=[r_sc, r_m], w=[rr])
                        yield
                        S.op("dve", lambda e: e.max(out=rv[:, c, 0:8], in_=Rc), r=[rr], w=[r_rv])
                        yield
                        S.op("dve", lambda e: e.match_replace(out=tmpR[:], in_to_replace=rv[:, c, 0:8], in_values=Rc, imm_value=-1.0), r=[rr, r_rv], w=[r_t])
                        yield
                        S.op("dve", lambda e: e.max(out=rv[:, c, 8:16], in_=tmpR[:]), r=[r_t], w=[r_rv])
                        yield
                        S.op("dve", lambda e: e.scalar_tensor_tensor(out=Rc, in0=Rc, scalar=rv[:, c, 15:16], in1=Rc, op0=ALU.is_ge, op1=ALU.mult),
                             r=[rr, r_rv], w=[rr])
                        yield
                    r_g = st["r_g"]
                    S.op("dve", lambda e: e.tensor_tensor(
                        out=G[:].rearrange("p (a b) -> p a b", a=16),
                        in0=rv[:, 0, :].rearrange("p (a o) -> p a o", o=1).to_broadcast([128, 16, 16]),
                        in1=rv[:, 1, :].rearrange("p (o b) -> p o b", o=1).to_broadcast([128, 16, 16]), op=ALU.mult),
                         r=[st["r_rv"][0], st["r_rv"][1]], w=[r_g])
                    yield
                    S.op("dve", lambda e: e.max(out=m8[:, 2, :], in_=G[:]), r=[r_g], w=[r_g])
                    yield
                    S.op("dve", lambda e: e.match_replace(out=G2[:], in_to_replace=m8[:, 2, :], in_values=G[:], imm_value=-1.0), r=[r_g], w=[r_g])
                    yield
                    S.op("dve", lambda e: e.max(out=m8[:, 3, :], in_=G2[:]), r=[r_g], w=[r_g])
                    yield
                    S.op("dve", lambda e: e.memset(zz[:, 0:1], 0.0), w=[r_g])
                    yield
                    S.op("dve", lambda e: e.scalar_tensor_tensor(out=G2[:], in0=G[:], scalar=m8[:, 3, 7:8], in1=G[:], op0=ALU.is_ge, op1=ALU.mult,
                                                                 accum_out=zz[:, 0:1]), r=[r_g], w=[r_g])
                    yield
                    S.op("dve", lambda e: e.reciprocal(out=zz[:, 1:2], in_=zz[:, 0:1]), r=[r_g], w=[r_g])
                    yield
                    S.op("dve", lambda e: e.tensor_scalar(out=RC[:, tt, h, 0, :], in0=RC[:, tt, h, 0, :], scalar1=zz[:, 1:2], scalar2=None, op0=ALU.mult),
                         r=[rr, r_g], w=[rr])
                    yield
                    S.op("dve", lambda e: e.tensor_scalar(out=tau2[:, tt * 8 + h:tt * 8 + h + 1], in0=m8[:, 3, 7:8], scalar1=zz[:, 1:2],
                                                          scalar2=0.99999, op0=ALU.mult, op1=ALU.mult), r=[r_g], w=[rr])
                    yield

                wload(wq[0], w_pq_v, 0, 256, r_wq[0])
                ustate = {"u": 0}

                def qproj(h):
                    b = h % 2
                    if h + 1 < 8:
                        wload(wq[1 - b], w_pq_v, (h + 1) * 256, 256, r_wq[1 - b])
                    for c in range(2):
                        qi = (h % 2) * 2 + c
                        for tg in range(2):
                            pb = ustate["u"] % 2
                            ustate["u"] += 1
                            for kc in range(KC):
                                S.op("pe", lambda e: e.matmul(
                                    psq[pb][:], lhsT=wq[b][:, kc, c * 128:(c + 1) * 128], rhs=h2T[:, kc, tg * 512:(tg + 1) * 512],
                                    start=(kc == 0), stop=(kc == KC - 1)), r=[r_wq[b]] + [r_h2[t_][kc] for t_ in range(tg * 4, tg * 4 + 4)], w=[r_psq[pb]])
                            S.op("act", lambda e: e.activation(out=qTb[qi][:, tg * 512:(tg + 1) * 512], in_=psq[pb][:], func=AF.Copy),
                                 r=[r_psq[pb]], w=[r_qT[qi]])

                qproj(0)
                for h in range(8):
                    if h + 1 < 8:
                        qproj(h + 1)
                    for t0 in range(0, 8, NCH):
                        gens = [chain(t0 + i, h, sets[i]) for i in range(NCH) if t0 + i < 8]
                        alive = list(gens)
                        while alive:
                            nxt = []
                            for g in alive:
                                try:
                                    next(g)
                                    nxt.append(g)
                                except StopIteration:
                                    pass
                            alive = nxt
                S.barrier()
            with ExitStack() as es2:
                def T2(name, shape, dt):
                    return es2.enter_context(nc.sbuf_tensor("e8_" + name, shape, dt))

                def P2(name, shape, dt):
                    return es2.enter_context(nc.psum_tensor("e8_" + name, shape, dt))

                NB = 8
                ub_ = [T2("ub%d" % i, [128, KC, 256], BF16) for i in range(2)]
                r_ub = [Res(), Res()]
                psA = [P2("psA%d" % i, [128, 512], F32) for i in range(2)]
                r_psA = [Res(), Res()]
                psW = [P2("psW%d" % i, [128, 512], F32) for i in range(2)]
                r_psW = [[Res() for _ in range(4)] for _ in range(2)]
                AgT = [T2("AgT%d" % i, [128, 512], F32) for i in range(2)]
                r_Ag = [Res(), Res()]
                Prb = [T2("Prb%d" % i, [128, 128], F32) for i in range(NB)]
                r_Pr = [Res() for _ in range(NB)]
                Mb = [T2("Mb%d" % i, [128, 128], BF16) for i in range(NB)]
                r_M = [Res() for _ in range(NB)]
                WgTb = [T2("WgTb%d" % i, [128, 512], BF16) for i in range(2)]
                r_WgT = [Res(), Res()]
                items = [(eg, tg, k) for eg in range(64) for tg in range(2) for k in range(2)]
                NI = len(items)

                def emit_A(n, kc):
                    eg, tg, k = items[n]
                    b = eg % 2
                    pa = n % 2
                    if kc == 0 and tg == 0 and k == 0 and eg + 1 < 64:
                        wload(ub_[1 - b], uT_v, (eg + 1) * 256, 256, r_ub[1 - b])
                    S.op("pe", lambda e: e.matmul(
                        psA[pa][:], lhsT=ub_[b][:, kc, k * 128:(k + 1) * 128], rhs=h2T[:, kc, tg * 512:(tg + 1) * 512],
                        start=(kc == 0), stop=(kc == KC - 1)),
                         r=[r_ub[b]] + [r_h2[t_][kc] for t_ in range(tg * 4, tg * 4 + 4)], w=[r_psA[pa]])

                def emit_gelu(n):
                    pa = n % 2
                    S.op("act", lambda e: e.activation(out=AgT[pa][:], in_=psA[pa][:], func=AF.Gelu), r=[r_psA[pa]], w=[r_Ag[pa]])

                wload(ub_[0], uT_v, 0, 256, r_ub[0])
                for kc in range(KC):
                    emit_A(0, kc)
                emit_gelu(0)
                pc = 0
                for n in range(NI):
                    eg, tg, k = items[n]
                    i = eg * 2 + k
                    pa = n % 2
                    for q in range(32):
                        tl, h = q // 8, q % 8
                        tt = tg * 4 + tl
                        pi = pc % NB
                        pc += 1
                        S.op("act", lambda e, pi=pi, tt=tt, h=h, i=i: e.mul(out=Prb[pi][:], in_=RC[:, tt, h, 1, :], mul=RC[:, tt, h, 0, i:i + 1]),
                             r=[r_RC[tt][h]], w=[r_Pr[pi]])
                        S.op("dve", lambda e, pi=pi, tt=tt, h=h: e.scalar_tensor_tensor(
                            out=Mb[pi][:], in0=Prb[pi][:], scalar=tau2[:, tt * 8 + h:tt * 8 + h + 1], in1=Prb[pi][:],
                            op0=ALU.is_ge, op1=ALU.mult), r=[r_Pr[pi], r_RC[tt][h]], w=[r_M[pi]])
                        if n + 1 < NI:
                            emit_A(n + 1, q)
                        S.op("pe", lambda e, pi=pi, pa=pa, tl=tl, h=h: e.matmul(
                            psW[pa][:, tl * 128:(tl + 1) * 128], lhsT=Mb[pi][:], rhs=identb[:], start=(h == 0), stop=(h == 7)),
                             r=[r_M[pi], r_const], w=[r_psW[pa][tl]])
                    if n + 1 < NI:
                        emit_gelu(n + 1)
                    S.op("dve", lambda e, pa=pa: e.tensor_tensor(out=WgTb[pa][:], in0=psW[pa][:], in1=AgT[pa][:], op=ALU.mult),
                         r=r_psW[pa] + [r_Ag[pa]], w=[r_WgT[pa]])
                    S.dma("sp", WgT_d[i * 128:(i + 1) * 128, tg * 512:(tg + 1) * 512], WgTb[pa][:], r=[r_WgT[pa]], w=[R_scr["WgT_d"]])
            S.barrier()
        if stop_after <= 4:
            S.finish()
            return nc

        with ExitStack() as es:
            def T(name, shape, dt):
                return es.enter_context(nc.sbuf_tensor("f_" + name, shape, dt))

            def P(name, shape, dt):
                return es.enter_context(nc.psum_tensor("f_" + name, shape, dt))

            g2rep = T("g2rep", [128, D], F32)
            r_g2 = Res()
            for i in range(4):
                S.dma("sp", g2rep[:, i * 1024:(i + 1) * 1024],
                      mod_d[160 + i * 8:160 + (i + 1) * 8, :].rearrange("(o j) p -> o (j p)", o=1).partition_broadcast(128),
                      r=[R_scr["mod_d"]], w=[r_g2])
            vbuf = [T("vbuf%d" % i, [128, 8, 512], BF16) for i in range(2)]
            wbuf = [T("wbuf%d" % i, [128, 8, S_Q], BF16) for i in range(2)]
            r_vbuf = [Res(), Res()]
            r_wbuf = [Res(), Res()]
            psY = [P("psY%d" % i, [128, 512], F32) for i in range(8)]
            r_psY = [Res() for _ in range(8)]
            xs = [T("xs%d" % i, [128, 512], F32) for i in range(2)]
            t1 = [T("t1%d" % i, [128, 512], F32) for i in range(2)]
            r_xs = [Res(), Res()]
            r_t1 = [Res(), Res()]
            pv_v = pv.rearrange("(et p) d -> p et d", p=128)
            WgT_v = WgT_d.rearrange("(et p) t -> p et t", p=128)

            def load_batch(dg, eb, b):
                for hlf in range(2):
                    S.dma("pool", vbuf[b][:, hlf * 4:(hlf + 1) * 4, :], pv_v[:, eb * 8 + hlf * 4:eb * 8 + (hlf + 1) * 4, dg * 512:(dg + 1) * 512], w=[r_vbuf[b]])
                for hlf in range(2):
                    S.dma("sp", wbuf[b][:, hlf * 4:(hlf + 1) * 4, :], WgT_v[:, eb * 8 + hlf * 4:eb * 8 + (hlf + 1) * 4, :], r=[R_scr["WgT_d"]], w=[r_wbuf[b]])

            seq = [(dg, eb) for dg in range(8) for eb in range(16)]
            load_batch(0, 0, 0)
            u = 0
            for si, (dg, eb) in enumerate(seq):
                b = si % 2
                if si + 1 < len(seq):
                    load_batch(seq[si + 1][0], seq[si + 1][1], 1 - b)
                for et in range(8):
                    for tt in range(8):
                        S.op("pe", lambda e, b=b, et=et, tt=tt, eb=eb: e.matmul(
                            psY[tt][:], lhsT=wbuf[b][:, et, tt * 128:(tt + 1) * 128], rhs=vbuf[b][:, et, :],
                            start=(eb == 0 and et == 0), stop=(eb == 15 and et == 7)), r=[r_vbuf[b], r_wbuf[b]], w=[r_psY[tt]])
                if eb == 15:
                    for tt in range(8):
                        pb = u % 2
                        u += 1
                        S.dma("sp", xs[pb][:], x1_d[tt * 128:(tt + 1) * 128, dg * 512:(dg + 1) * 512], r=[R_scr["x1_d"]], w=[r_xs[pb]])
                        S.op("dve", lambda e, pb=pb, tt=tt, dg=dg: e.tensor_tensor(out=t1[pb][:], in0=psY[tt][:], in1=g2rep[:, dg * 512:(dg + 1) * 512], op=ALU.mult),
                             r=[r_psY[tt], r_g2], w=[r_t1[pb]])
                        S.op("dve", lambda e, pb=pb: e.tensor_tensor(out=t1[pb][:], in0=t1[pb][:], in1=xs[pb][:], op=ALU.add),
                             r=[r_t1[pb], r_xs[pb]], w=[r_t1[pb]])
                        S.dma("sp", out[tt * 128:(tt + 1) * 128, dg * 512:(dg + 1) * 512], t1[pb][:], r=[r_t1[pb]], w=[R_scr["out"]])
        S.finish()
    return nc


def host_inputs(x, c, w_ada, b_ada, norm1_g, w_in, b_f, q_norm_moba, k_norm_moba, q_norm_fox, k_norm_fox,
                w_out, norm2_g, w_pq, peer_sub_keys, peer_u, peer_v, cores=None):
    f = np.float32
    x = np.asarray(x, f)
    c = np.asarray(c, f)
    shared = {
        "w_ada": np.ascontiguousarray(np.asarray(w_ada, f)[0]),
        "b_ada": np.ascontiguousarray(np.asarray(b_ada, f)[0][None, :]),
        "n1g": np.ascontiguousarray(np.asarray(norm1_g, f)[0].reshape(KC, 128).T),
        "n2g": np.ascontiguousarray(np.asarray(norm2_g, f)[0].reshape(KC, 128).T),
        "w_in": np.ascontiguousarray(np.asarray(w_in, f)[0]),
        "bfrep": np.ascontiguousarray(np.broadcast_to(np.asarray(b_f, f)[0][None, :], (128, 16))),
        "gains": np.ascontiguousarray(np.stack([np.asarray(q_norm_moba, f)[0], np.asarray(k_norm_moba, f)[0],
                                                np.asarray(q_norm_fox, f)[0], np.asarray(k_norm_fox, f)[0]], axis=1)),
        "w_out": np.ascontiguousarray(np.asarray(w_out, f)[0]),
        "w_pq": np.ascontiguousarray(np.asarray(w_pq, f)[0]),
        "skT": np.ascontiguousarray(np.asarray(peer_sub_keys, f)[0].transpose(3, 0, 1, 2).reshape(128, 2048)),
        "uT": np.ascontiguousarray(np.asarray(peer_u, f)[0].T),
        "pv": np.ascontiguousarray(np.asarray(peer_v, f)[0]),
    }
    past = np.zeros((128, 8, 8), f)
    for r in range(8):
        past[:, r, r:] = -1e30
    shared["pastb"] = past.reshape(128, 64)
    En = np.zeros((8, 8, 128), f)
    for n in range(8):
        En[n, n, :] = 1.0
    shared["En"] = En.reshape(8, 1024)
    slopes = 2.0 ** (-8.0 * np.arange(1, 17, dtype=np.float64) / 16)
    p = np.arange(128)
    tri = np.where(p[:, None] <= p[None, :], 0.0, NEG).astype(f)
    in_maps = []
    for cid in (range(8) if cores is None else cores):
        b, hf = cid // 2, cid % 2
        gt = [gtile(r, hf) for r in range(8)]
        xb = x[b]
        xq = np.concatenate([xb[g * 128:(g + 1) * 128] for g in gt], axis=0)
        al = np.zeros((128, 16, 72), np.float64)
        sel = np.zeros((128, 8, 16), f)
        mk = np.zeros((128, 16, 128), f)
        for r in range(8):
            g = gt[r]
            tmid = 128 * g + 64
            sel[:, r, g] = 1.0
            for j in range(2 * r + 2):
                al[:, :, OFF[r] + j] = slopes[None, :] * (128 * j + p[:, None] - tmid)
            if g == 2 * r:
                mk[:, 2 * r, :] = tri
                mk[:, 2 * r + 1, :] = NEG
            else:
                mk[:, 2 * r, :] = 0.0
                mk[:, 2 * r + 1, :] = tri
        m = dict(shared)
        m["xall"] = np.ascontiguousarray(xb)
        m["xq"] = np.ascontiguousarray(xq)
        m["cT"] = np.ascontiguousarray(c[b].reshape(KC, 128).T)
        m["albias"] = np.ascontiguousarray(al.astype(f).reshape(128, 16 * 72))
        m["sel"] = np.ascontiguousarray(sel.reshape(128, 128))
        m["maskb"] = np.ascontiguousarray(mk.reshape(128, 2048))
        in_maps.append(m)
    return in_maps


def kernel(**inputs):
    in_maps = host_inputs(**inputs)
    nc = build()
    res = run_bass_kernel_spmd(nc, in_maps, core_ids=list(range(8)))
    B, Sq = 4, 2048
    outf = np.zeros((B, Sq, D), np.float32)
    for cid in range(8):
        b, hf = cid // 2, cid % 2
        o = np.asarray(res.results[cid]["out"])
        for r in range(8):
            g = gtile(r, hf)
            outf[b, g * 128:(g + 1) * 128] = o[r * 128:(r + 1) * 128]
    return outf
```
